# Optimizing a Trainium2 kernel written in Bass

```python
import jax, jax.numpy as jnp
from jax import lax
import numpy as np

D_MODEL = 2048
BATCH = 8
SEQ = 2048
DEPTH = 1

N_META = 16
CONV_K = 4
EPS = 1e-6
SSD_D_INNER = D_MODEL
SSD_HEAD_DIM = 64
SSD_HEADS = SSD_D_INNER // SSD_HEAD_DIM
SSD_GROUPS = 8
SSD_HPG = SSD_HEADS // SSD_GROUPS
SSD_STATE = 128
SSD_CHUNK = 128
SSD_CONV_DIM = SSD_D_INNER + 2 * SSD_GROUPS * SSD_STATE
LRU_WIDTH = D_MODEL
LRU_HEADS = 8
LRU_BLOCK = LRU_WIDTH // LRU_HEADS
LRU_C = 8.0
N_BRANCH = 2
COL_Z = SSD_D_INNER
COL_XBC = COL_Z + SSD_CONV_DIM
COL_DT = COL_XBC + SSD_HEADS
COL_LX = COL_DT + LRU_WIDTH
COL_LY = COL_LX + LRU_WIDTH
IN_COLS = COL_LY + N_BRANCH * D_MODEL
MOE_GROUPS = 8
MOE_EXP_PER_GROUP = 8
MOE_EXPERTS = MOE_GROUPS * MOE_EXP_PER_GROUP
MOE_TOPK = 2
MOE_FF = D_MODEL // 2
MOE_BLOCK = 128

kernel_name = "hybrid_ssd_rglru_hmoe_block"


def _rmsnorm(x, g):
    xf = x.astype(jnp.float32)
    y = xf * lax.rsqrt(jnp.mean(xf * xf, axis=-1, keepdims=True) + EPS)
    return (y * g.astype(jnp.float32)).astype(x.dtype)


def _causal_dwconv(x, w, b):
    k = w.shape[0]
    l = x.shape[1]
    xp = jnp.pad(x, ((0, 0), (k - 1, 0), (0, 0)))
    out = b
    for i in range(k):
        out = out + xp[:, i:i + l] * w[i]
    return out


def _ssd_branch(z, xbc, dt_raw, conv_w, conv_b, dt_bias, a_log, d_skip, norm_w):
    b, l, _ = xbc.shape
    xbc = jax.nn.silu(_causal_dwconv(xbc, conv_w, conv_b))
    xs, bm, cm = jnp.split(xbc, [SSD_D_INNER, SSD_D_INNER + SSD_GROUPS * SSD_STATE], axis=-1)
    xs = xs.reshape(b, l, SSD_HEADS, SSD_HEAD_DIM)
    dt = jax.nn.softplus((dt_raw + dt_bias).astype(jnp.float32))
    a = -jnp.exp(a_log.astype(jnp.float32))
    pad = (-l) % SSD_CHUNK
    lp = l + pad
    nc = lp // SSD_CHUNK
    q = SSD_CHUNK

    def front(t):
        return jnp.pad(t, ((0, 0), (pad, 0)) + ((0, 0),) * (t.ndim - 2))

    xdt = front(xs * dt[..., None]).reshape(b, nc, q, SSD_GROUPS, SSD_HPG, SSD_HEAD_DIM)
    bmc = front(bm).reshape(b, nc, q, SSD_GROUPS, SSD_STATE)
    cmc = front(cm).reshape(b, nc, q, SSD_GROUPS, SSD_STATE)
    a_dt = jnp.moveaxis(front(dt * a).reshape(b, nc, q, SSD_GROUPS, SSD_HPG), 2, -1)
    a_cum = jnp.cumsum(a_dt, axis=-1)
    causal = jnp.tril(jnp.ones((q, q), dtype=bool))
    seg = jnp.where(causal, a_cum[..., :, None] - a_cum[..., None, :], -jnp.inf)
    decay_in = jnp.exp(seg)
    cb = jnp.einsum('bclgn,bcsgn->bcgls', cmc, bmc)
    y_diag = jnp.einsum('bcgls,bcgjls,bcsgjp->bclgjp', cb, decay_in, xdt)
    decay_to_end = jnp.exp(a_cum[..., -1:] - a_cum)
    states = jnp.einsum('bcsgn,bcgjs,bcsgjp->bcgjpn', bmc, decay_to_end, xdt)
    chunk_decay = jnp.exp(a_cum[..., -1])

    def step(carry, inp):
        st, dec = inp
        return carry * dec[..., None, None] + st, carry

    _, prev = lax.scan(step, jnp.zeros_like(states[:, 0]),
                       (jnp.moveaxis(states, 1, 0), jnp.moveaxis(chunk_decay, 1, 0)))
    prev = jnp.moveaxis(prev, 0, 1)
    y_off = jnp.einsum('bclgn,bcgjpn,bcgjl->bclgjp', cmc, prev, jnp.exp(a_cum))
    y = (y_diag + y_off).reshape(b, lp, SSD_HEADS, SSD_HEAD_DIM)[:, pad:]
    y = y + d_skip[:, None] * xs
    y = y.reshape(b, l, SSD_D_INNER) * jax.nn.silu(z)
    yg = y.reshape(b, l, SSD_GROUPS, SSD_D_INNER // SSD_GROUPS).astype(jnp.float32)
    yg = yg * lax.rsqrt(jnp.mean(yg * yg, axis=-1, keepdims=True) + EPS)
    return (yg.reshape(b, l, SSD_D_INNER) * norm_w).astype(z.dtype)


def _rglru_branch(xb, yb, conv_w, conv_b, wa, ba, wx, bx, lam):
    b, l, _ = xb.shape
    xr = _causal_dwconv(xb, conv_w, conv_b)
    xh = xr.reshape(b, l, LRU_HEADS, LRU_BLOCK)
    gate_r = jax.nn.sigmoid((jnp.einsum('blhi,hij->blhj', xh, wa).reshape(b, l, LRU_WIDTH) + ba).astype(jnp.float32))
    gate_i = jax.nn.sigmoid((jnp.einsum('blhi,hij->blhj', xh, wx).reshape(b, l, LRU_WIDTH) + bx).astype(jnp.float32))
    log_a = -LRU_C * gate_r * jax.nn.softplus(-lam.astype(jnp.float32))
    a = jnp.exp(log_a)
    mult = jnp.sqrt(-jnp.expm1(2.0 * log_a))
    u = mult * gate_i * xr.astype(jnp.float32)

    def combine(left, right):
        a_l, u_l = left
        a_r, u_r = right
        return a_l * a_r, a_r * u_l + u_r

    _, h = lax.associative_scan(combine, (a, u), axis=1)
    return (h * jax.nn.gelu(yb.astype(jnp.float32))).astype(xb.dtype)


def _moe(u, w_rg, w_re, w1, w3, w2):
    b, l, d = u.shape
    t = u.reshape(b * l, d)
    n_tok = t.shape[0]
    g_prob = jax.nn.softmax((t @ w_rg).astype(jnp.float32), axis=-1)
    g_p, g_idx = lax.top_k(g_prob, 1)
    e_logits = (t @ w_re).reshape(n_tok, MOE_GROUPS, MOE_EXP_PER_GROUP)
    e_sel = jnp.take_along_axis(e_logits, g_idx[:, :, None], axis=1)[:, 0]
    e_prob = jax.nn.softmax(e_sel.astype(jnp.float32), axis=-1)
    e_p, e_loc = lax.top_k(e_prob, MOE_TOPK)
    gates = g_p * e_p / jnp.sum(e_p, axis=-1, keepdims=True)
    expert_id = g_idx * MOE_EXP_PER_GROUP + e_loc

    n_asg = n_tok * MOE_TOPK
    n_blocks = -(-(n_asg + MOE_EXPERTS * (MOE_BLOCK - 1)) // MOE_BLOCK)
    p_len = n_blocks * MOE_BLOCK
    flat_e = expert_id.reshape(-1)
    flat_tok = jnp.repeat(jnp.arange(n_tok, dtype=jnp.int32), MOE_TOPK)
    flat_gate = gates.reshape(-1)
    order = jnp.argsort(flat_e)
    se = flat_e[order]
    counts = jnp.bincount(flat_e, length=MOE_EXPERTS)
    padded = (counts + MOE_BLOCK - 1) // MOE_BLOCK * MOE_BLOCK
    pad_end = jnp.cumsum(padded)
    pad_start = pad_end - padded
    start = jnp.cumsum(counts) - counts
    dest = pad_start[se] + jnp.arange(n_asg) - start[se]
    buf_tok = jnp.full((p_len,), n_tok, dtype=jnp.int32).at[dest].set(flat_tok[order])
    buf_gate = jnp.zeros((p_len,), u.dtype).at[dest].set(flat_gate[order].astype(u.dtype))
    blk_e = jnp.minimum(jnp.searchsorted(pad_end, jnp.arange(n_blocks) * MOE_BLOCK, side='right'),
                        MOE_EXPERTS - 1)
    t_pad = jnp.concatenate([t, jnp.zeros((1, d), t.dtype)], axis=0)

    def expert_block(args):
        tok, gate, e = args
        xb = t_pad[tok]
        hdn = jax.nn.silu(xb @ w1[e]) * (xb @ w3[e])
        return (hdn @ w2[e]) * gate[:, None]

    y_blocks = lax.map(expert_block, (buf_tok.reshape(n_blocks, MOE_BLOCK),
                                      buf_gate.reshape(n_blocks, MOE_BLOCK), blk_e))
    out = jnp.zeros((n_tok + 1, d), u.dtype).at[buf_tok].add(y_blocks.reshape(p_len, d).astype(u.dtype))
    return out[:n_tok].reshape(b, l, d)


def setup_inputs(seed: int = 0) -> dict:
    key = jax.random.key(seed)
    ks = jax.random.split(key, 32)
    f32 = jnp.float32

    def nrm(k, shape, scale):
        return jax.random.normal(k, shape, f32) * scale

    dt0 = jnp.exp(jax.random.uniform(ks[6], (DEPTH, SSD_HEADS), f32, np.log(1e-3), np.log(1e-1)))
    a_pow = jax.random.uniform(ks[14], (DEPTH, LRU_WIDTH), f32, 0.9, 0.999)
    a_base = a_pow ** (1.0 / LRU_C)
    return {
        "x": nrm(ks[0], (BATCH, SEQ, D_MODEL), 1.0),
        "meta_tokens": nrm(ks[1], (N_META, D_MODEL), 1.0),
        "norm_mix": 1.0 + nrm(ks[2], (DEPTH, D_MODEL), 0.02),
        "w_in": nrm(ks[3], (DEPTH, D_MODEL, IN_COLS), D_MODEL ** -0.5),
        "ssd_conv_w": nrm(ks[4], (DEPTH, CONV_K, SSD_CONV_DIM), CONV_K ** -0.5),
        "ssd_conv_b": nrm(ks[5], (DEPTH, SSD_CONV_DIM), 0.01),
        "ssd_dt_bias": dt0 + jnp.log(-jnp.expm1(-dt0)),
        "ssd_a_log": jnp.log(jax.random.uniform(ks[7], (DEPTH, SSD_HEADS), f32, 1.0, 16.0)),
        "ssd_d": 1.0 + nrm(ks[8], (DEPTH, SSD_HEADS), 0.02),
        "ssd_norm": 1.0 + nrm(ks[9], (DEPTH, SSD_D_INNER), 0.02),
        "w_ssd_out": nrm(ks[10], (DEPTH, SSD_D_INNER, D_MODEL), SSD_D_INNER ** -0.5),
        "lru_conv_w": nrm(ks[11], (DEPTH, CONV_K, LRU_WIDTH), CONV_K ** -0.5),
        "lru_conv_b": nrm(ks[12], (DEPTH, LRU_WIDTH), 0.01),
        "lru_wa": nrm(ks[13], (DEPTH, LRU_HEADS, LRU_BLOCK, LRU_BLOCK), LRU_BLOCK ** -0.5),
        "lru_ba": nrm(ks[15], (DEPTH, LRU_WIDTH), 0.01),
        "lru_wx": nrm(ks[16], (DEPTH, LRU_HEADS, LRU_BLOCK, LRU_BLOCK), LRU_BLOCK ** -0.5),
        "lru_bx": nrm(ks[17], (DEPTH, LRU_WIDTH), 0.01),
        "lru_lambda": jnp.log(a_base) - jnp.log1p(-a_base),
        "w_lru_out": nrm(ks[18], (DEPTH, LRU_WIDTH, D_MODEL), LRU_WIDTH ** -0.5),
        "gate_bias": nrm(ks[19], (DEPTH, N_BRANCH, D_MODEL), 0.01),
        "w_out": nrm(ks[20], (DEPTH, D_MODEL, D_MODEL), D_MODEL ** -0.5),
        "norm_ffn": 1.0 + nrm(ks[21], (DEPTH, D_MODEL), 0.02),
        "w_router_group": nrm(ks[22], (DEPTH, D_MODEL, MOE_GROUPS), D_MODEL ** -0.5),
        "w_router_expert": nrm(ks[23], (DEPTH, D_MODEL, MOE_EXPERTS), D_MODEL ** -0.5),
        "w_exp_gate": nrm(ks[24], (DEPTH, MOE_EXPERTS, D_MODEL, MOE_FF), D_MODEL ** -0.5),
        "w_exp_up": nrm(ks[25], (DEPTH, MOE_EXPERTS, D_MODEL, MOE_FF), D_MODEL ** -0.5),
        "w_exp_down": nrm(ks[26], (DEPTH, MOE_EXPERTS, MOE_FF, D_MODEL), MOE_FF ** -0.5),
        "norm_final": 1.0 + nrm(ks[27], (D_MODEL,), 0.02),
    }


def reference(x, meta_tokens, norm_mix, w_in, ssd_conv_w, ssd_conv_b, ssd_dt_bias, ssd_a_log,
              ssd_d, ssd_norm, w_ssd_out, lru_conv_w, lru_conv_b, lru_wa, lru_ba, lru_wx, lru_bx,
              lru_lambda, w_lru_out, gate_bias, w_out, norm_ffn, w_router_group, w_router_expert,
              w_exp_gate, w_exp_up, w_exp_down, norm_final):
    b = x.shape[0]
    meta = jnp.broadcast_to(meta_tokens.astype(x.dtype)[None], (b, N_META, D_MODEL))
    h = jnp.concatenate([meta, x], axis=1)
    l = h.shape[1]
    for layer in range(DEPTH):
        u = _rmsnorm(h, norm_mix[layer])
        proj = u @ w_in[layer]
        z, xbc, dt_raw, lx, ly, gl = jnp.split(proj, [COL_Z, COL_XBC, COL_DT, COL_LX, COL_LY], axis=-1)
        y_ssd = _ssd_branch(z, xbc, dt_raw, ssd_conv_w[layer], ssd_conv_b[layer], ssd_dt_bias[layer],
                            ssd_a_log[layer], ssd_d[layer], ssd_norm[layer]) @ w_ssd_out[layer]
        y_lru = _rglru_branch(lx, ly, lru_conv_w[layer], lru_conv_b[layer], lru_wa[layer], lru_ba[layer],
                              lru_wx[layer], lru_bx[layer], lru_lambda[layer]) @ w_lru_out[layer]
        gates = jax.nn.sigmoid((gl.reshape(b, l, N_BRANCH, D_MODEL) + gate_bias[layer]).astype(jnp.float32)).astype(h.dtype)
        mixed = gates[..., 0, :] * y_ssd + gates[..., 1, :] * y_lru
        h = h + mixed @ w_out[layer]
        h = h + _moe(_rmsnorm(h, norm_ffn[layer]), w_router_group[layer], w_router_expert[layer],
                     w_exp_gate[layer], w_exp_up[layer], w_exp_down[layer])
    return _rmsnorm(h, norm_final)[:, N_META:]
```

```python
import os
import numpy as np
from contextlib import ExitStack
import concourse.bass as bass
import concourse.mybir as mybir
from concourse.bass_utils import run_bass_kernel_spmd

F32 = mybir.dt.float32
BF16 = mybir.dt.bfloat16
I32 = mybir.dt.int32
ALU = mybir.AluOpType
AF = mybir.ActivationFunctionType
AX = mybir.AxisListType

ENGS = ("pe", "act", "dve", "pool", "sp")


class Prog:
    def __init__(self, nc, stack):
        self.nc = nc
        self.stack = stack
        self.ops = {e: [] for e in ENGS}
        self.cnt = {e: 0 for e in ENGS}
        self.seen = {e: {} for e in ENGS}
        self.sem = {e: stack.enter_context(nc.semaphore("sem_" + e)) for e in ENGS}
        self.w = {}
        self.r = {}
        self.dsem = {}
        self.unsig = {e: False for e in ENGS}

    def dma_sem(self, name):
        if name not in self.dsem:
            self.dsem[name] = [self.stack.enter_context(self.nc.semaphore("d_" + name)), 0]
        return self.dsem[name]

    def _deps(self, eng, reads, writes, acc):
        toks = []
        for res in reads:
            toks += self.w.get(res, [])
        for res in writes:
            toks += self.w.get(res, [])
            toks += self.r.get(res, [])
        for res in acc:
            toks += self.r.get(res, [])
        seen = self.seen[eng]
        best = {}
        for t in toks:
            key = (t[0], t[1])
            if t[2] <= seen.get(key, 0):
                continue
            if t[2] > best.get(key, 0):
                best[key] = t[2]
        waits = []
        for key, v in best.items():
            seen[key] = v
            waits.append((key, v))
        return waits

    def _commit(self, tok, reads, writes, acc):
        for res in reads:
            self.r.setdefault(res, []).append(tok)
        for res in writes:
            self.w[res] = [tok]
            self.r[res] = []
        for res in acc:
            self.w.setdefault(res, []).append(tok)

    def op(self, eng, fn, reads=(), writes=(), acc=(), sig=True):
        waits = self._deps(eng, reads, writes, acc)
        n = self.cnt[eng] + 1
        if sig:
            self.cnt[eng] = n
        self.unsig[eng] = not sig
        self._commit(("e", eng, n), reads, writes, acc)
        self.ops[eng].append((waits, fn, sig, None))

    def dma(self, eng, semname, fn, reads=(), writes=(), acc=()):
        waits = self._deps(eng, reads, writes, acc)
        s = self.dma_sem(semname)
        s[1] += 16
        self._commit(("d", semname, s[1]), reads, writes, acc)
        self.ops[eng].append((waits, fn, False, s[0]))

    def wait_all(self, eng, resources):
        waits = self._deps(eng, resources, (), ())
        self.ops[eng].append((waits, None, False, None))

    def barrier(self):
        for e in ENGS:
            assert not self.unsig[e], e
        for e in ENGS:
            waits = []
            seen = self.seen[e]
            for f in ENGS:
                if f != e and self.cnt[f] > seen.get(("e", f), 0):
                    seen[("e", f)] = self.cnt[f]
                    waits.append((("e", f), self.cnt[f]))
            for name, (s, v) in self.dsem.items():
                if v > seen.get(("d", name), 0):
                    seen[("d", name)] = v
                    waits.append((("d", name), v))
            self.ops[e].append((waits, None, False, None))

    def emit(self):
        nc = self.nc
        with nc.Block() as block:
            def run(eng_name):
                def body(e):
                    for waits, fn, sig, dsem in self.ops[eng_name]:
                        for key, v in waits:
                            if key[0] == "e":
                                e.wait_ge(self.sem[key[1]], v)
                            else:
                                e.wait_ge(self.dsem[key[1]][0], v)
                        if fn is None:
                            continue
                        ins = fn(e)
                        if dsem is not None:
                            ins.then_inc(dsem, 16)
                        elif sig:
                            ins.then_inc(self.sem[eng_name], 1)
                return body
            block.tensor(run("pe"))
            block.scalar(run("act"))
            block.vector(run("dve"))
            block.gpsimd(run("pool"))
            block.sync(run("sp"))


D = 2048
NMETA = 16
SEQ = 2048
TB = 512
NBLK = SEQ // TB
COL_Z, COL_XS, COL_B, COL_C, COL_DT, COL_LX, COL_LY, COL_G0, COL_G1, INC = 0, 2048, 4096, 5120, 6144, 6176, 8224, 10272, 12320, 14368
NE = 64
FF = 1024
CAP = 128
EPS = 1e-6

PC = {}
_off = 0
for _n, _c in (("gmix", 16), ("scw", 128), ("scb", 32), ("snw", 16), ("sd", 16), ("lcw", 64), ("lcb", 16), ("lba", 16),
               ("lbx", 16), ("llam", 16), ("gb0", 16), ("gb1", 16), ("dtb", 1)):
    PC[_n] = _off
    _off += _c
NPC = _off


class Ring:
    def __init__(self, P, slots, prefix):
        self.P, self.slots, self.prefix, self.n = P, slots, prefix, 0

    def load(self, srcs, k, n):
        i = self.n % len(self.slots)
        self.n += 1
        view = self.slots[i][:, 0:k * n].rearrange("p (k n) -> p k n", k=k)
        res = f"{self.prefix}{i}"
        for q, (k0, k1, src) in enumerate(srcs):
            self.P.dma("pool", res, lambda e, k0=k0, k1=k1, src=src: e.dma_start(out=view[:, k0:k1, :], in_=src),
                       writes=[res] if q == 0 else [], acc=[res] if q else [])
        return view, res


def run_jobs(ring, jobs):
    nw = len(ring.slots)
    wj = [j for j in jobs if j[0] is not None]
    loaded = {}
    nxt = 0
    wi = 0
    for j in jobs:
        if j[0] is not None:
            while nxt < len(wj) and nxt <= wi + nw - 1:
                jj = wj[nxt]
                loaded[id(jj)] = ring.load(jj[0], jj[1], jj[2])
                nxt += 1
            v, r = loaded.pop(id(j))
            wi += 1
            j[3](v, r)
        else:
            j[3](None, None)


def build(debug=False, nblk=NBLK, do_moe=True):
    nc = bass.Bass("TRN2", target_bir_lowering=False)
    dt_ = nc.dram_tensor
    x_d = dt_("x", [SEQ, D], F32, kind="ExternalInput").ap()
    meta_d = dt_("meta", [128, D], F32, kind="ExternalInput").ap()
    pvec_d = dt_("pvec", [128, NPC], F32, kind="ExternalInput").ap()
    alog_d = dt_("alog_bc", [128, 32], F32, kind="ExternalInput").ap()
    gffn_d = dt_("gffn_bc", [128, D], F32, kind="ExternalInput").ap()
    gfin_d = dt_("gfin_bc", [128, D], F32, kind="ExternalInput").ap()
    wr_d = dt_("wr", [D, 72], F32, kind="ExternalInput").ap()
    win_d = dt_("w_in", [D, INC], F32, kind="ExternalInput").ap()
    wso_d = dt_("w_ssd_out", [D, D], F32, kind="ExternalInput").ap()
    wlo_d = dt_("w_lru_out", [D, D], F32, kind="ExternalInput").ap()
    wo_d = dt_("w_out", [D, D], F32, kind="ExternalInput").ap()
    wa_d = dt_("lru_wa", [8, 256, 256], F32, kind="ExternalInput").ap()
    wx_d = dt_("lru_wx", [8, 256, 256], F32, kind="ExternalInput").ap()
    if do_moe:
        w1_d = dt_("w1", [NE, D, FF], F32, kind="ExternalInput").ap()
        w3_d = dt_("w3", [NE, D, FF], F32, kind="ExternalInput").ap()
        w2_d = dt_("w2", [NE, FF, D], F32, kind="ExternalInput").ap()
    out_d = dt_("out", [SEQ, D], F32, kind="ExternalOutput").ap()
    h1_d = dt_("h1s", [SEQ, D], F32, kind="Internal").ap()
    xb_d = dt_("xbuf", [NE * CAP, D], BF16, kind="Internal").ap()
    yb_d = dt_("ybuf", [NE * CAP, D], F32, kind="Internal").ap()
    dbg = {}
    if debug:
        dbg["ssdT"] = dt_("dbg_ssdT", [128, 16, TB], F32, kind="ExternalOutput").ap()
        dbg["lruT"] = dt_("dbg_lruT", [128, 16, TB], F32, kind="ExternalOutput").ap()
        dbg["mixT"] = dt_("dbg_mixT", [128, 16, TB], F32, kind="ExternalOutput").ap()
        dbg["h1"] = dt_("dbg_h1", [SEQ, D], F32, kind="ExternalOutput").ap()
        dbg["route"] = dt_("dbg_route", [128, 16, 4], F32, kind="ExternalOutput").ap()

    with ExitStack() as st:
        P = Prog(nc, st)
        sb = lambda name, shape, dt: st.enter_context(nc.sbuf_tensor("s_" + name, shape, dt))
        pv = sb("pv", [128, NPC], F32)
        alog = sb("alog", [128, 32], F32)
        Aneg = sb("Aneg", [128, 32], F32)
        klam = sb("klam", [128, 16], F32)
        identf = sb("identf", [128, 128], F32)
        identb = sb("identb", [128, 128], BF16)
        triU = sb("triU", [128, 128], F32)
        triS = sb("triS", [128, 128], F32)
        negm = sb("negm", [128, 128], F32)
        onesf = sb("onesf", [128, 128], F32)
        iof = sb("iof", [128, 128], F32)
        pidx = sb("pidx", [128, 1], F32)
        eoff = sb("eoff", [128, 64], F32)
        wr = sb("wr", [128, 16, 72], F32)
        gffn = sb("gffn", [128, D], F32)
        ctail = sb("ctail", [128, 48, 3], F32)
        hst = sb("hst", [128, 16], F32)
        ST = sb("ST", [128, 32, 64], F32)
        STb = sb("STb", [128, 32, 64], BF16)
        basebc = sb("basebc", [128, 64], F32)
        IDX = sb("IDX", [128, 16, 2], I32)
        GATE = sb("GATE", [128, 16, 2], F32)
        ps0 = st.enter_context(nc.psum_tensor("ps0", [128, 1024], BF16))
        ps = [None] + [st.enter_context(nc.psum_tensor(f"ps{i}", [128, 512], F32)) for i in range(1, 8)]

        P.dma("sp", "c_pv", lambda e: e.dma_start(out=pv[:], in_=pvec_d), writes=["pv"])
        P.dma("sp", "c_alog", lambda e: e.dma_start(out=alog[:], in_=alog_d), writes=["alog"])
        P.dma("sp", "c_gffn", lambda e: e.dma_start(out=gffn[:], in_=gffn_d), writes=["gffn"])
        P.dma("sp", "c_wr", lambda e: e.dma_start(out=wr[:], in_=wr_d.rearrange("(k p) n -> p k n", p=128)), writes=["wr"])
        P.op("pool", lambda e: e.iota(iof[:], pattern=[[1, 128]], base=0, channel_multiplier=0, allow_small_or_imprecise_dtypes=True), writes=["iof"])
        P.op("pool", lambda e: e.iota(pidx[:], pattern=[[0, 1]], base=0, channel_multiplier=1, allow_small_or_imprecise_dtypes=True), writes=["pidx"])
        P.op("dve", lambda e: e.tensor_scalar(out=identf[:], in0=iof[:], scalar1=pidx[:, 0:1], scalar2=None, op0=ALU.is_equal), reads=["iof", "pidx"], writes=["identf"])
        P.op("dve", lambda e: e.tensor_copy(out=identb[:], in_=identf[:]), reads=["identf"], writes=["identb"])
        P.op("dve", lambda e: e.tensor_scalar(out=triU[:], in0=iof[:], scalar1=pidx[:, 0:1], scalar2=None, op0=ALU.is_ge), reads=["iof", "pidx"], writes=["triU"])
        P.op("dve", lambda e: e.tensor_scalar(out=triS[:], in0=iof[:], scalar1=pidx[:, 0:1], scalar2=None, op0=ALU.is_gt), reads=["iof", "pidx"], writes=["triS"])
        P.op("dve", lambda e: e.tensor_scalar(out=negm[:], in0=triU[:], scalar1=-1.0, scalar2=1e30, op0=ALU.add, op1=ALU.mult), reads=["triU"], writes=["negm"])
        P.op("dve", lambda e: e.memset(onesf[:], 1.0), writes=["onesf"])
        tmask = sb("tmask", [128, 128], F32)
        P.op("dve", lambda e: e.tensor_scalar(out=tmask[:], in0=iof[:], scalar1=float(128 - NMETA), scalar2=None, op0=ALU.is_ge), reads=["iof"], writes=["tmask"])
        P.op("dve", lambda e: e.tensor_scalar(out=eoff[:], in0=iof[:, 0:64], scalar1=float(CAP), scalar2=None, op0=ALU.mult), reads=["iof"], writes=["eoff"])
        P.op("dve", lambda e: e.memset(ctail[:], 0.0), writes=["ctail"])
        P.op("dve", lambda e: e.memset(hst[:], 0.0), writes=["hst"])
        P.op("dve", lambda e: e.memset(ST[:], 0.0), writes=["ST"])
        P.op("dve", lambda e: e.memset(STb[:], 0.0), writes=["STb"])
        P.op("dve", lambda e: e.memset(basebc[:], 0.0), writes=["basebc"])
        P.op("act", lambda e: e.activation(out=Aneg[:], in_=alog[:], func=AF.Exp), reads=["alog"], writes=["Aneg"])
        P.op("dve", lambda e: e.tensor_scalar(out=Aneg[:], in0=Aneg[:], scalar1=-1.0, scalar2=None, op0=ALU.mult), reads=["Aneg"], writes=["Aneg"])
        c_lam = PC["llam"]
        P.op("act", lambda e: e.activation(out=klam[:], in_=pv[:, c_lam:c_lam + 16], func=AF.Exp, scale=-1.0), reads=["pv"], writes=["klam"])
        P.op("act", lambda e: e.activation(out=klam[:], in_=klam[:], func=AF.Ln, bias=onesf[:, 0:1]), reads=["klam", "onesf"], writes=["klam"])
        P.op("dve", lambda e: e.tensor_scalar(out=klam[:], in0=klam[:], scalar1=-8.0, scalar2=None, op0=ALU.mult), reads=["klam"], writes=["klam"])

        WT = 256
        winv = win_d.rearrange("(k p) n -> p k n", p=128)

        def win_tile(c0, n=WT):
            return [(0, 16, winv[:, :, c0:c0 + n])]

        def sq_tile(w_d, c0):
            return [(0, 16, w_d.rearrange("(k p) n -> p k n", p=128)[:, :, c0:c0 + WT])]

        with ExitStack() as st2:
            sb2 = lambda name, shape, dt: st2.enter_context(nc.sbuf_tensor("s_" + name, shape, dt))
            ring = Ring(P, [sb2(f"wsl{i}", [128, 16 * WT], BF16) for i in range(3)], "w")
            uT = sb2("uT", [128, 16, TB], BF16)
            ssdT = sb2("ssdT", [128, 16, TB], BF16)
            lruT = sb2("lruT", [128, 16, TB], BF16)
            mixT = sb2("mixT", [128, 16, TB], BF16)
            xt = sb2("xt", [128, D], F32)
            xn = sb2("xn", [128, D], BF16)
            xsub = [sb2(f"xsub{i}", [128, WT], F32) for i in range(2)]
            u2T = sb2("u2T", [128, 8, 128], F32)
            sm = sb2("sm", [128, 32], F32)
            rt = sb2("rt", [128, 10, 72], F32)
            BTs = sb2("BTs", [128, 2, TB], BF16)
            CTs = sb2("CTs", [128, 2, TB], BF16)
            xsT = sb2("xsT", [128, 2, TB], BF16)
            yT = sb2("yT", [128, 2, TB], F32)
            dtT = sb2("dtT", [32, TB], F32)
            cbuf = sb2("cbuf", [128, TB + 3], F32)
            cacc = sb2("cacc", [128, TB], F32)
            xrb = sb2("xrb", [128, 2, TB], BF16)
            glb = sb2("glb", [128, 2, TB], BF16)
            Gs = sb2("Gs", [128, 2, TB], BF16)
            tl = [sb2(f"tl{i}", [128, TB], F32) for i in range(5)]
            dtk = sb2("dtk", [128, 4, 32], F32)
            adt = sb2("adt", [128, 4, 32], F32)
            acum = sb2("acum", [128, 4, 32], F32)
            dte = sb2("dte", [128, 4, 32], F32)
            eend = sb2("eend", [128, 4, 32], F32)
            xdt = sb2("xdt", [128, 256], BF16)
            xdtw = sb2("xdtw", [128, 256], BF16)
            Btok = sb2("Btok", [128, 128], BF16)
            Rm = sb2("Rm", [128, 512], F32)
            dm = sb2("dm", [128, 512], F32)
            MT = sb2("MT", [128, 512], BF16)
            eB = sb2("eB", [128, 512], F32)
            CdT = sb2("CdT", [128, 512], BF16)
            cbs = sb2("cbs", [128, 128], F32)
            v4 = lambda ap: ap.rearrange("p (j l) -> p j l", j=4)

            def rms_rstd(src_ap, q, scratch_ap, scr_res, col, tag):
                P.op("act", lambda e: e.activation(out=scratch_ap, in_=src_ap, func=AF.Square, accum_out=sm[0:q, col:col + 1]),
                     reads=[tag], writes=[scr_res, "sm"])
                P.op("dve", lambda e: e.tensor_scalar(out=sm[0:q, col:col + 1], in0=sm[0:q, col:col + 1], scalar1=1.0 / D, scalar2=EPS,
                                                      op0=ALU.mult, op1=ALU.add), reads=["sm"], writes=["sm"])
                P.op("act", lambda e: e.activation(out=sm[0:q, col:col + 1], in_=sm[0:q, col:col + 1], func=AF.Sqrt), reads=["sm"], writes=["sm"])
                P.op("dve", lambda e: e.reciprocal(out=sm[0:q, col:col + 1], in_=sm[0:q, col:col + 1]), reads=["sm"], writes=["sm"])

            def mixer_block(blk):
                is_meta = blk < 0
                tb = 128 if is_meta else TB
                nq = 1 if is_meta else TB // 128
                Q = 128
                jobs = []

                def build_u(_v, _r):
                    for ti in range(nq):
                        src = meta_d if is_meta else x_d[blk * TB + ti * 128: blk * TB + (ti + 1) * 128, :]
                        P.dma("sp", "xt", lambda e, src=src: e.dma_start(out=xt[0:Q, :], in_=src), writes=["xt"])
                        rms_rstd(xt[0:Q, :], Q, xn[0:Q, :], "xn", 0, "xt")
                        P.op("act", lambda e: e.activation(out=xn[0:Q, :], in_=xt[0:Q, :], func=AF.Identity, scale=sm[0:Q, 0:1]),
                             reads=["xt", "sm"], writes=["xn"])
                        for half in range(2):
                            for j in range(8):
                                k = half * 8 + j
                                P.op("pe", lambda e, k=k, j=j: e.transpose(out=ps0[:, j * 128:j * 128 + Q], in_=xn[0:Q, k * 128:(k + 1) * 128],
                                                                       identity=identb[0:Q, 0:Q]),
                                     reads=["xn", "identb"], writes=["ps0"] if j == 0 else [], acc=["ps0"] if j else [], sig=(j == 7))
                            g0 = PC["gmix"] + half * 8
                            P.op("dve", lambda e, half=half, ti=ti, g0=g0: e.tensor_tensor(
                                out=uT[:, half * 8:half * 8 + 8, ti * 128:ti * 128 + Q],
                                in0=ps0[:, :].rearrange("p (j t) -> p j t", j=8)[:, :, 0:Q],
                                in1=pv[:, g0:g0 + 8].unsqueeze(2).to_broadcast([128, 8, Q]), op=ALU.mult),
                                reads=["ps0", "pv"], acc=["uT"])
                jobs.append((None, 0, 0, build_u))

                def proj(view, wres, j, ncol, bank):
                    bres = f"ps{bank}"
                    for k in range(16):
                        P.op("pe", lambda e, k=k: e.matmul(ps[bank][0:ncol, 0:tb], lhsT=view[:, k, j * 128:j * 128 + ncol], rhs=uT[:, k, 0:tb],
                                                           start=(k == 0), stop=(k == 15)),
                             reads=[wres, "uT"], writes=[bres] if k == 0 else [], acc=[bres] if k else [], sig=(k == 15))

                def conv_chunk(bank, tcol, wcol0, wstride, bcol, out_ap, out_res, func):
                    bres = f"ps{bank}"
                    P.op("act", lambda e: e.activation(out=cbuf[:, 3:3 + tb], in_=ps[bank][:, 0:tb], func=AF.Copy), reads=[bres], writes=["cbuf"])
                    P.op("act", lambda e: e.activation(out=cbuf[:, 0:3], in_=ctail[:, tcol, :], func=AF.Copy), reads=["ctail"], acc=["cbuf"])
                    P.op("act", lambda e: e.activation(out=cacc[:, 0:tb], in_=cbuf[:, 0:tb], func=AF.Identity, scale=pv[:, wcol0:wcol0 + 1],
                                                       bias=pv[:, bcol:bcol + 1]),
                         reads=["cbuf", "pv"], writes=["cacc"])
                    for kk in (1, 2, 3):
                        last = (kk == 3 and func is None)
                        P.op("dve", lambda e, kk=kk, last=last: e.scalar_tensor_tensor(
                            out=(out_ap if last else cacc[:, 0:tb]), in0=cbuf[:, kk:kk + tb],
                            scalar=pv[:, wcol0 + kk * wstride:wcol0 + kk * wstride + 1], in1=cacc[:, 0:tb], op0=ALU.mult, op1=ALU.add),
                            reads=["cbuf", "cacc", "pv"], writes=[] if last else ["cacc"], acc=[out_res] if last else [])
                    if func is not None:
                        P.op("act", lambda e: e.activation(out=out_ap, in_=cacc[:, 0:tb], func=func), reads=["cacc"], acc=[out_res])
                    P.op("act", lambda e: e.activation(out=ctail[:, tcol, :], in_=cbuf[:, tb:tb + 3], func=AF.Copy), reads=["cbuf"], writes=["ctail"])

                def f_ly(hd):
                    def fn(view, wres):
                        for jc in range(2):
                            bank = 1 + jc
                            proj(view, wres, jc, 128, bank)
                            P.op("act", lambda e, bank=bank, jc=jc: e.activation(out=glb[:, jc, 0:tb], in_=ps[bank][:, 0:tb], func=AF.Gelu_apprx_tanh),
                                 reads=[f"ps{bank}"], acc=["glb"])
                    return fn

                def f_lx(hd):
                    def fn(view, wres):
                        for ic in range(2):
                            c = hd * 2 + ic
                            bank = 1 + ic
                            proj(view, wres, ic, 128, bank)
                            conv_chunk(bank, 32 + c, PC["lcw"] + c, 16, PC["lcb"] + c, xrb[:, ic, 0:tb], "xrb", None)
                    return fn

                def f_wg(hd):
                    def fn(view, wres):
                        r_, ig_, a_, m_, hs_ = tl
                        for jc in range(2):
                            c = hd * 2 + jc
                            for gi, bank in ((0, 3), (1, 4)):
                                for ic in range(2):
                                    P.op("pe", lambda e, gi=gi, ic=ic, bank=bank, jc=jc: e.matmul(
                                        ps[bank][:, 0:tb], lhsT=view[:, gi * 2 + ic, jc * 128:(jc + 1) * 128], rhs=xrb[:, ic, 0:tb],
                                        start=(ic == 0), stop=(ic == 1)),
                                        reads=[wres, "xrb"], writes=[f"ps{bank}"] if ic == 0 else [], acc=[f"ps{bank}"] if ic else [], sig=(ic == 1))
                            P.op("act", lambda e, c=c: e.activation(out=r_[:, 0:tb], in_=ps[3][:, 0:tb], func=AF.Sigmoid, bias=pv[:, PC["lba"] + c:PC["lba"] + c + 1]),
                                 reads=["ps3", "pv"], writes=["tl0"])
                            P.op("act", lambda e, c=c: e.activation(out=ig_[:, 0:tb], in_=ps[4][:, 0:tb], func=AF.Sigmoid, bias=pv[:, PC["lbx"] + c:PC["lbx"] + c + 1]),
                                 reads=["ps4", "pv"], writes=["tl1"])
                            P.op("act", lambda e, c=c: e.activation(out=a_[:, 0:tb], in_=r_[:, 0:tb], func=AF.Exp, scale=klam[:, c:c + 1]),
                                 reads=["tl0", "klam"], writes=["tl2"])
                            P.op("dve", lambda e: e.tensor_tensor(out=m_[:, 0:tb], in0=a_[:, 0:tb], in1=a_[:, 0:tb], op=ALU.mult), reads=["tl2"], writes=["tl3"])
                            P.op("dve", lambda e: e.tensor_scalar(out=m_[:, 0:tb], in0=m_[:, 0:tb], scalar1=-1.0, scalar2=1.0, op0=ALU.mult, op1=ALU.add),
                                 reads=["tl3"], writes=["tl3"])
                            P.op("act", lambda e: e.activation(out=m_[:, 0:tb], in_=m_[:, 0:tb], func=AF.Sqrt), reads=["tl3"], writes=["tl3"])
                            P.op("dve", lambda e: e.tensor_tensor(out=m_[:, 0:tb], in0=m_[:, 0:tb], in1=ig_[:, 0:tb], op=ALU.mult), reads=["tl3", "tl1"], writes=["tl3"])
                            P.op("dve", lambda e, jc=jc: e.tensor_tensor(out=m_[:, 0:tb], in0=m_[:, 0:tb], in1=xrb[:, jc, 0:tb], op=ALU.mult), reads=["tl3", "xrb"], writes=["tl3"])
                            if is_meta:
                                P.op("dve", lambda e: e.tensor_tensor(out=m_[:, 0:tb], in0=m_[:, 0:tb], in1=tmask[:, :], op=ALU.mult), reads=["tl3", "tmask"], writes=["tl3"])
                            P.op("dve", lambda e, c=c: e.tensor_tensor_scan(out=hs_[:, 0:tb], data0=a_[:, 0:tb], data1=m_[:, 0:tb], initial=hst[:, c:c + 1],
                                                                            op0=ALU.mult, op1=ALU.add), reads=["tl2", "tl3", "hst"], writes=["tl4"])
                            P.op("act", lambda e, c=c: e.activation(out=hst[:, c:c + 1], in_=hs_[:, tb - 1:tb], func=AF.Copy), reads=["tl4"], writes=["hst"])
                            if not is_meta:
                                P.op("dve", lambda e, c=c, jc=jc: e.tensor_tensor(out=lruT[:, c, 0:tb], in0=hs_[:, 0:tb], in1=glb[:, jc, 0:tb], op=ALU.mult),
                                     reads=["tl4", "glb"], acc=["lruT"])
                    return fn

                for hd in range(8):
                    if not is_meta:
                        jobs.append((win_tile(COL_LY + hd * 256), 16, WT, f_ly(hd)))
                    jobs.append((win_tile(COL_LX + hd * 256), 16, WT, f_lx(hd)))
                    wg_srcs = [(0, 2, wa_d[hd].rearrange("(ic p) j -> p ic j", p=128)), (2, 4, wx_d[hd].rearrange("(ic p) j -> p ic j", p=128))]
                    jobs.append((wg_srcs, 4, 256, f_wg(hd)))

                def f_dt(view, wres):
                    for k in range(16):
                        P.op("pe", lambda e, k=k: e.matmul(ps[5][0:32, 0:tb], lhsT=view[:, k, 0:32], rhs=uT[:, k, 0:tb], start=(k == 0), stop=(k == 15)),
                             reads=[wres, "uT"], writes=["ps5"] if k == 0 else [], acc=["ps5"] if k else [], sig=(k == 15))
                    P.op("act", lambda e: e.activation(out=dtT[:, 0:tb], in_=ps[5][0:32, 0:tb], func=AF.Exp, bias=pv[0:32, PC["dtb"]:PC["dtb"] + 1]),
                         reads=["ps5", "pv"], writes=["dtT"])
                    P.op("act", lambda e: e.activation(out=dtT[:, 0:tb], in_=dtT[:, 0:tb], func=AF.Ln, bias=onesf[0:32, 0:1]), reads=["dtT", "onesf"], writes=["dtT"])
                    if is_meta:
                        P.op("dve", lambda e: e.tensor_tensor(out=dtT[:, 0:tb], in0=dtT[:, 0:tb], in1=tmask[0:32, :], op=ALU.mult), reads=["dtT", "tmask"], writes=["dtT"])
                    for q in range(nq):
                        P.op("pe", lambda e, q=q: e.transpose(out=ps[5][0:Q, 0:32], in_=dtT[:, q * 128:q * 128 + Q], identity=identf[0:32, 0:32]),
                             reads=["dtT", "identf"], writes=["ps5"])
                        P.op("act", lambda e, q=q: e.activation(out=dtk[0:Q, q, :], in_=ps[5][0:Q, 0:32], func=AF.Copy), reads=["ps5"], acc=["dtk"])
                        P.op("dve", lambda e, q=q: e.tensor_tensor(out=adt[0:Q, q, :], in0=ps[5][0:Q, 0:32], in1=Aneg[0:Q, :], op=ALU.mult),
                             reads=["ps5", "Aneg"], acc=["adt"])
                        P.op("pe", lambda e, q=q: e.matmul(ps[6][0:Q, 0:32], lhsT=triU[0:Q, 0:Q], rhs=adt[0:Q, q, :], start=True, stop=True),
                             reads=["adt", "triU"], writes=["ps6"])
                        P.op("pe", lambda e, q=q: e.matmul(ps[6][:, 32:64], lhsT=onesf[0:Q, :], rhs=adt[0:Q, q, :], start=True, stop=True),
                             reads=["adt", "onesf"], acc=["ps6"])
                        P.op("act", lambda e, q=q: e.activation(out=acum[0:Q, q, :], in_=ps[6][0:Q, 0:32], func=AF.Copy), reads=["ps6"], acc=["acum"])
                        P.op("dve", lambda e, q=q: e.tensor_tensor(out=dte[0:Q, q, :], in0=ps[6][0:Q, 32:64], in1=acum[0:Q, q, :], op=ALU.subtract),
                             reads=["ps6", "acum"], acc=["dte"])
                        P.op("act", lambda e, q=q: e.activation(out=dte[0:Q, q, :], in_=dte[0:Q, q, :], func=AF.Exp), reads=["dte"], acc=["dte"])
                        P.op("act", lambda e, q=q: e.activation(out=eend[:, q, :], in_=ps[6][:, 32:64], func=AF.Exp), reads=["ps6"], acc=["eend"])
                jobs.append((win_tile(COL_DT, 32), 16, 32, f_dt))

                def f_bc(which, gp):
                    dst = BTs if which == 0 else CTs
                    dres = "BTs" if which == 0 else "CTs"

                    def fn(view, wres):
                        for j in range(2):
                            c = (16 if which == 0 else 24) + gp * 2 + j
                            bank = 1 + j
                            proj(view, wres, j, 128, bank)
                            conv_chunk(bank, c, PC["scw"] + c, 32, PC["scb"] + c, dst[:, j, 0:tb], dres, AF.Silu)
                    return fn

                def f_xs(g):
                    def fn(view, wres):
                        for j in range(2):
                            c = g * 2 + j
                            bank = 1 + j
                            proj(view, wres, j, 128, bank)
                            conv_chunk(bank, c, PC["scw"] + c, 32, PC["scb"] + c, xsT[:, j, 0:tb], "xsT", AF.Silu)
                    return fn

                def ssd_group(g, gl_, zview, zres):
                    _ks = int(os.environ.get("KSSD", "100"))
                    for q in range(nq):
                        tsl = slice(q * 128, q * 128 + Q)
                        for jj in range(2):
                            P.op("pe", lambda e, tsl=tsl, jj=jj: e.transpose(out=ps0[0:Q, jj * 128:(jj + 1) * 128], in_=xsT[:, jj, tsl], identity=identb[:, :]),
                                 reads=["xsT", "identb"], writes=["ps0"] if jj == 0 else [], acc=["ps0"] if jj else [], sig=False)
                        P.op("pe", lambda e, tsl=tsl: e.transpose(out=ps0[0:Q, 256:384], in_=BTs[:, gl_, tsl], identity=identb[:, :]),
                             reads=["BTs", "identb"], acc=["ps0"])
                        if _ks <= 1:
                            return
                        P.op("dve", lambda e, q=q: e.tensor_tensor(out=xdt[0:Q, :].rearrange("p (h d) -> p h d", h=4),
                                                                   in0=ps0[0:Q, 0:256].rearrange("p (h d) -> p h d", h=4),
                                                                   in1=dtk[0:Q, q, 4 * g:4 * g + 4].unsqueeze(2).to_broadcast([Q, 4, 64]), op=ALU.mult),
                             reads=["ps0", "dtk"], writes=["xdt"])
                        if _ks <= 2:
                            return
                        P.op("dve", lambda e, q=q: e.tensor_tensor(out=xdtw[0:Q, :].rearrange("p (h d) -> p h d", h=4),
                                                                   in0=xdt[0:Q, :].rearrange("p (h d) -> p h d", h=4),
                                                                   in1=dte[0:Q, q, 4 * g:4 * g + 4].unsqueeze(2).to_broadcast([Q, 4, 64]), op=ALU.mult),
                             reads=["xdt", "dte"], writes=["xdtw"])
                        if _ks <= 3:
                            return
                        P.op("dve", lambda e: e.tensor_copy(out=Btok[0:Q, :], in_=ps0[0:Q, 256:384]), reads=["ps0"], writes=["Btok"])
                        if _ks <= 4:
                            return
                        if not is_meta:
                            P.op("pe", lambda e, tsl=tsl: e.matmul(ps[5][:, 0:128], lhsT=BTs[:, gl_, tsl], rhs=CTs[:, gl_, tsl], start=True, stop=True),
                                 reads=["BTs", "CTs"], writes=["ps5"])
                            P.op("act", lambda e: e.activation(out=cbs[:, :], in_=ps[5][:, 0:128], func=AF.Copy), reads=["ps5"], writes=["cbs"])
                            P.op("dve", lambda e, q=q: e.tensor_tensor(out=v4(Rm[:, :]),
                                                                       in0=adt[:, q, 4 * g:4 * g + 4].unsqueeze(2).to_broadcast([128, 4, 128]),
                                                                       in1=triU[:, :].unsqueeze(1).to_broadcast([128, 4, 128]), op=ALU.mult),
                                 reads=["adt", "triU"], writes=["Rm"])
                            P.op("pe", lambda e: e.matmul(ps[6][:, :], lhsT=onesf[:, :], rhs=Rm[:, :], start=True, stop=True), reads=["Rm", "onesf"], writes=["ps6"])
                            P.op("dve", lambda e, q=q: e.tensor_tensor(out=v4(dm[:, :]), in0=v4(ps[6][:, :]),
                                                                       in1=acum[:, q, 4 * g:4 * g + 4].unsqueeze(2).to_broadcast([128, 4, 128]), op=ALU.subtract),
                                 reads=["ps6", "acum"], writes=["dm"])
                            P.op("dve", lambda e: e.tensor_tensor(out=v4(dm[:, :]), in0=v4(dm[:, :]),
                                                                  in1=negm[:, :].unsqueeze(1).to_broadcast([128, 4, 128]), op=ALU.add),
                                 reads=["dm", "negm"], writes=["dm"])
                            P.op("act", lambda e: e.activation(out=dm[:, :], in_=dm[:, :], func=AF.Exp), reads=["dm"], writes=["dm"])
                            P.op("dve", lambda e: e.tensor_tensor(out=v4(MT[:, :]), in0=v4(dm[:, :]),
                                                                  in1=cbs[:, :].unsqueeze(1).to_broadcast([128, 4, 128]), op=ALU.mult),
                                 reads=["dm", "cbs"], writes=["MT"])
                            P.op("act", lambda e: e.activation(out=eB[:, :], in_=ps[6][:, :], func=AF.Exp), reads=["ps6"], writes=["eB"])
                            P.op("dve", lambda e, tsl=tsl: e.tensor_tensor(out=v4(CdT[:, :]), in0=v4(eB[:, :]),
                                                                  in1=CTs[:, gl_, tsl].unsqueeze(1).to_broadcast([128, 4, 128]), op=ALU.mult),
                                 reads=["eB", "CTs"], writes=["CdT"])
                            for j in range(4):
                                h = 4 * g + j
                                po = (j % 2) * 64
                                bank = 3 + j // 2
                                first = (j % 2 == 0)
                                P.op("pe", lambda e, j=j, po=po, bank=bank: e.matmul(ps[bank][po:po + 64, 0:128], lhsT=xdt[:, j * 64:(j + 1) * 64],
                                                                                      rhs=MT[:, j * 128:(j + 1) * 128], start=True, stop=False),
                                     reads=["xdt", "MT"], writes=[f"ps{bank}"] if first else [], acc=[] if first else [f"ps{bank}"], sig=False)
                                P.op("pe", lambda e, j=j, po=po, bank=bank, h=h: e.matmul(ps[bank][po:po + 64, 0:128], lhsT=STb[:, h, :],
                                                                                           rhs=CdT[:, j * 128:(j + 1) * 128], start=False, stop=True),
                                     reads=["STb", "CdT"], acc=[f"ps{bank}"], sig=(j % 2 == 1))
                            for yc in range(2):
                                cch = 2 * g + yc
                                P.op("dve", lambda e, tsl=tsl, yc=yc, cch=cch: e.scalar_tensor_tensor(out=yT[:, yc, tsl], in0=xsT[:, yc, tsl],
                                                                                             scalar=pv[:, PC["sd"] + cch:PC["sd"] + cch + 1],
                                                                                             in1=ps[3 + yc][:, 0:128], op0=ALU.mult, op1=ALU.add),
                                     reads=[f"ps{3 + yc}", "xsT", "pv"], acc=["yT"])
                        P.op("pe", lambda e: e.matmul(ps[7][:, 0:256], lhsT=Btok[0:Q, :], rhs=xdtw[0:Q, :], start=True, stop=True),
                             reads=["Btok", "xdtw"], writes=["ps7"])
                        if _ks <= 5:
                            return
                        P.op("dve", lambda e, q=q: e.tensor_tensor(out=ST[:, 4 * g:4 * g + 4, :], in0=ST[:, 4 * g:4 * g + 4, :],
                                                                   in1=eend[:, q, 4 * g:4 * g + 4].unsqueeze(2).to_broadcast([128, 4, 64]), op=ALU.mult),
                             reads=["ST", "eend"], writes=["ST"])
                        P.op("dve", lambda e: e.tensor_tensor(out=ST[:, 4 * g:4 * g + 4, :], in0=ST[:, 4 * g:4 * g + 4, :],
                                                              in1=ps[7][:, 0:256].rearrange("p (h d) -> p h d", h=4), op=ALU.add),
                             reads=["ST", "ps7"], writes=["ST"])
                        P.op("act", lambda e: e.activation(out=STb[:, 4 * g:4 * g + 4, :], in_=ST[:, 4 * g:4 * g + 4, :], func=AF.Copy), reads=["ST"], writes=["STb"])
                    if is_meta:
                        return
                    for yc in range(2):
                        bank = 1 + yc
                        proj(zview, zres, yc, 128, bank)
                        P.op("act", lambda e, bank=bank: e.activation(out=tl[0][:, 0:tb], in_=ps[bank][:, 0:tb], func=AF.Silu), reads=[f"ps{bank}"], writes=["tl0"])
                        P.op("dve", lambda e, yc=yc: e.tensor_tensor(out=yT[:, yc, 0:tb], in0=yT[:, yc, 0:tb], in1=tl[0][:, 0:tb], op=ALU.mult),
                             reads=["yT", "tl0"], writes=["yT"])
                        P.op("act", lambda e, yc=yc: e.activation(out=tl[1 + yc][:, 0:tb], in_=yT[:, yc, 0:tb], func=AF.Square), reads=["yT"], writes=[f"tl{1 + yc}"])
                    for yc in range(2):
                        P.op("pe", lambda e, yc=yc: e.matmul(ps[5][:, 0:tb], lhsT=onesf[:, :], rhs=tl[1 + yc][:, 0:tb], start=(yc == 0), stop=(yc == 1)),
                             reads=[f"tl{1 + yc}", "onesf"], writes=["ps5"] if yc == 0 else [], acc=["ps5"] if yc else [], sig=(yc == 1))
                    P.op("dve", lambda e: e.tensor_scalar(out=tl[3][:, 0:tb], in0=ps[5][:, 0:tb], scalar1=1.0 / 256.0, scalar2=EPS, op0=ALU.mult, op1=ALU.add),
                         reads=["ps5"], writes=["tl3"])
                    P.op("act", lambda e: e.activation(out=tl[3][:, 0:tb], in_=tl[3][:, 0:tb], func=AF.Sqrt), reads=["tl3"], writes=["tl3"])
                    P.op("dve", lambda e: e.reciprocal(out=tl[3][:, 0:tb], in_=tl[3][:, 0:tb]), reads=["tl3"], writes=["tl3"])
                    for yc in range(2):
                        cch = 2 * g + yc
                        P.op("dve", lambda e, yc=yc, cch=cch: e.scalar_tensor_tensor(out=ssdT[:, cch, 0:tb], in0=yT[:, yc, 0:tb],
                                                                                     scalar=pv[:, PC["snw"] + cch:PC["snw"] + cch + 1],
                                                                                     in1=tl[3][:, 0:tb], op0=ALU.mult, op1=ALU.mult),
                             reads=["yT", "tl3", "pv"], acc=["ssdT"])

                def f_z(g, gl_):
                    return lambda view, wres: ssd_group(g, gl_, view, wres)

                for gp in range(4):
                    jobs.append((win_tile(COL_B + gp * 256), 16, WT, f_bc(0, gp)))
                    jobs.append((win_tile(COL_C + gp * 256), 16, WT, f_bc(1, gp)))
                    for gg in range(2):
                        g = gp * 2 + gg
                        jobs.append((win_tile(COL_XS + g * 256), 16, WT, f_xs(g)))
                        if is_meta:
                            jobs.append((None, 0, 0, f_z(g, gg)))
                        else:
                            jobs.append((win_tile(COL_Z + g * 256), 16, WT, f_z(g, gg)))

                if not is_meta:
                    def f_gate(i, which):
                        def fn(view, wres):
                            for j in range(2):
                                dc = i * 2 + j
                                bank = 1 + j
                                proj(view, wres, j, 128, bank)
                                bcol = PC["gb0" if which == 0 else "gb1"] + dc
                                P.op("act", lambda e, bank=bank, j=j, bcol=bcol: e.activation(out=Gs[:, j, 0:tb], in_=ps[bank][:, 0:tb], func=AF.Sigmoid,
                                                                                              bias=pv[:, bcol:bcol + 1]), reads=[f"ps{bank}", "pv"], acc=["Gs"])
                        return fn

                    def f_yo(i, which):
                        src, sres = (ssdT, "ssdT") if which == 0 else (lruT, "lruT")

                        def fn(view, wres):
                            for j in range(2):
                                dc = i * 2 + j
                                bank = 3 + j
                                for k in range(16):
                                    P.op("pe", lambda e, k=k, bank=bank, j=j: e.matmul(ps[bank][:, 0:tb], lhsT=view[:, k, j * 128:(j + 1) * 128],
                                                                                     rhs=src[:, k, 0:tb], start=(k == 0), stop=(k == 15)),
                                         reads=[wres, sres], writes=[f"ps{bank}"] if k == 0 else [], acc=[f"ps{bank}"] if k else [], sig=(k == 15))
                                if which == 0:
                                    P.op("dve", lambda e, j=j, dc=dc, bank=bank: e.tensor_tensor(out=mixT[:, dc, 0:tb], in0=Gs[:, j, 0:tb], in1=ps[bank][:, 0:tb], op=ALU.mult),
                                         reads=["Gs", f"ps{bank}"], acc=["mixT"])
                                else:
                                    P.op("dve", lambda e, j=j, bank=bank: e.tensor_tensor(out=tl[j][:, 0:tb], in0=Gs[:, j, 0:tb], in1=ps[bank][:, 0:tb], op=ALU.mult),
                                         reads=["Gs", f"ps{bank}"], writes=[f"tl{j}"])
                                    P.op("dve", lambda e, j=j, dc=dc: e.tensor_tensor(out=mixT[:, dc, 0:tb], in0=mixT[:, dc, 0:tb], in1=tl[j][:, 0:tb], op=ALU.add),
                                         reads=[f"tl{j}", "mixT"], acc=["mixT"])
                        return fn
                    for i in range(8):
                        jobs.append((win_tile(COL_G0 + i * 256), 16, WT, f_gate(i, 0)))
                        jobs.append((sq_tile(wso_d, i * 256), 16, WT, f_yo(i, 0)))
                        jobs.append((win_tile(COL_G1 + i * 256), 16, WT, f_gate(i, 1)))
                        jobs.append((sq_tile(wlo_d, i * 256), 16, WT, f_yo(i, 1)))

                    def f_o(db):
                        def fn(view, wres):
                            for tt in range(4):
                                tg = blk * 4 + tt
                                xs_ = xsub[tt % 2]
                                xres = f"xsub{tt % 2}"
                                bank = 1 + (tt % 2)
                                P.dma("sp", xres, lambda e, xs_=xs_, tg=tg: e.dma_start(out=xs_[:, :], in_=x_d[tg * 128:(tg + 1) * 128, db * WT:(db + 1) * WT]),
                                      writes=[xres])
                                for k in range(16):
                                    P.op("pe", lambda e, k=k, bank=bank, tt=tt: e.matmul(ps[bank][:, 0:WT], lhsT=mixT[:, k, tt * 128:(tt + 1) * 128], rhs=view[:, k, :],
                                                                                       start=(k == 0), stop=(k == 15)),
                                         reads=[wres, "mixT"], writes=[f"ps{bank}"] if k == 0 else [], acc=[f"ps{bank}"] if k else [], sig=(k == 15))
                                P.op("dve", lambda e, xs_=xs_, bank=bank: e.tensor_tensor(out=xs_[:, :], in0=xs_[:, :], in1=ps[bank][:, 0:WT], op=ALU.add),
                                     reads=[xres, f"ps{bank}"], writes=[xres])
                                P.dma("sp", "h1st" + xres, lambda e, xs_=xs_, tg=tg: e.dma_start(out=h1_d[tg * 128:(tg + 1) * 128, db * WT:(db + 1) * WT], in_=xs_[:, :]),
                                      reads=[xres], acc=["h1d"])
                        return fn
                    for db in range(8):
                        jobs.append((sq_tile(wo_d, db * WT), 16, WT, f_o(db)))

                    def f_route(_v, _r):
                        for tt in range(4):
                            tg = blk * 4 + tt
                            P.dma("sp", "xt", lambda e, tg=tg: e.dma_start(out=xt[:, :], in_=h1_d[tg * 128:(tg + 1) * 128, :]), reads=["h1d"], writes=["xt"])
                            if debug:
                                P.dma("sp", "dbgh1", lambda e, tg=tg: e.dma_start(out=dbg["h1"][tg * 128:(tg + 1) * 128, :], in_=xt[:, :]), reads=["xt"], acc=["dbgh1"])
                            if do_moe:
                                route_tile(tg)
                    jobs.append((None, 0, 0, f_route))

                _km = int(os.environ.get("KMAXJOBS", "100000"))
                run_jobs(ring, jobs[:_km])

            def route_tile(tg):
                rms_rstd(xt[:, :], 128, xn[:, :], "xn", 1, "xt")
                P.op("dve", lambda e: e.scalar_tensor_tensor(out=xt[:, :], in0=xt[:, :], scalar=sm[:, 1:2], in1=gffn[:, :], op0=ALU.mult, op1=ALU.mult),
                     reads=["xt", "sm", "gffn"], writes=["xt"])
                P.op("act", lambda e: e.activation(out=xn[:, :], in_=xt[:, :], func=AF.Copy), reads=["xt"], writes=["xn"])
                for hf in range(2):
                    for qd in range(2):
                        for j in range(4):
                            k = hf * 8 + qd * 4 + j
                            P.op("pe", lambda e, k=k, j=j: e.transpose(out=ps[5][:, j * 128:(j + 1) * 128], in_=xt[:, k * 128:(k + 1) * 128], identity=identf[:, :]),
                                 reads=["xt", "identf"], writes=["ps5"] if j == 0 else [], acc=["ps5"] if j else [], sig=(j == 3))
                        P.op("act", lambda e, qd=qd: e.activation(out=u2T[:, qd * 4:(qd + 1) * 4, :], in_=ps[5][:, :].rearrange("p (j t) -> p j t", j=4), func=AF.Copy),
                             reads=["ps5"], acc=["u2T"])
                    for kk in range(8):
                        k = hf * 8 + kk
                        P.op("pe", lambda e, k=k, kk=kk: e.matmul(ps[6][:, 0:72], lhsT=u2T[:, kk, :], rhs=wr[:, k, :], start=(k == 0), stop=(k == 15)),
                             reads=["u2T", "wr"], writes=["ps6"] if k == 0 else [], acc=["ps6"] if k else [], sig=(kk == 7))
                lg = rt[:, 0, :]
                tmp = rt[:, 1, 0:64]
                esel, oh1, es2, oh2, goh = rt[:, 2, 0:8], rt[:, 2, 8:16], rt[:, 2, 16:24], rt[:, 2, 24:32], rt[:, 2, 32:40]
                A1, A2, Asum, pos = rt[:, 3, 0:64], rt[:, 4, 0:64], rt[:, 5, 0:64], rt[:, 6, 0:64]
                s = lambda c: sm[:, c:c + 1]
                RT = ["rt"]

                def dv(fn, extra=()):
                    wx = [x for x in extra if x in ("IDX", "GATE")]
                    P.op("dve", fn, reads=RT + list(extra), writes=RT, acc=wx)

                P.op("act", lambda e: e.activation(out=lg, in_=ps[6][:, 0:72], func=AF.Copy), reads=["ps6"], writes=RT)
                dv(lambda e: e.reduce_max(out=s(8), in_=lg[:, 0:8], axis=AX.X))
                dv(lambda e: e.tensor_scalar(out=goh, in0=lg[:, 0:8], scalar1=s(8), scalar2=None, op0=ALU.is_equal))
                dv(lambda e: e.tensor_scalar(out=s(9), in0=s(8), scalar1=-1.0, scalar2=None, op0=ALU.mult))
                P.op("act", lambda e: e.activation(out=tmp[:, 0:8], in_=lg[:, 0:8], func=AF.Exp, bias=s(9), accum_out=s(10)), reads=RT, writes=RT)
                dv(lambda e: e.reciprocal(out=s(10), in_=s(10)))
                dv(lambda e: e.tensor_tensor(out=tmp.rearrange("p (g x) -> p g x", g=8), in0=lg[:, 8:72].rearrange("p (g x) -> p g x", g=8),
                                             in1=goh.unsqueeze(2).to_broadcast([128, 8, 8]), op=ALU.mult))
                dv(lambda e: e.reduce_sum(out=esel, in_=tmp.rearrange("p (g x) -> p x g", g=8), axis=AX.X))
                dv(lambda e: e.reduce_max(out=s(11), in_=esel, axis=AX.X))
                dv(lambda e: e.tensor_scalar(out=oh1, in0=esel, scalar1=s(11), scalar2=None, op0=ALU.is_equal))
                dv(lambda e: e.scalar_tensor_tensor(out=es2, in0=oh1, scalar=-1e30, in1=esel, op0=ALU.mult, op1=ALU.add))
                dv(lambda e: e.reduce_max(out=s(12), in_=es2, axis=AX.X))
                dv(lambda e: e.tensor_scalar(out=oh2, in0=es2, scalar1=s(12), scalar2=None, op0=ALU.is_equal))
                dv(lambda e: e.tensor_tensor(out=s(13), in0=s(12), in1=s(11), op=ALU.subtract))
                P.op("act", lambda e: e.activation(out=s(13), in_=s(13), func=AF.Exp), reads=RT, writes=RT)
                dv(lambda e: e.tensor_scalar(out=s(14), in0=s(13), scalar1=1.0, scalar2=None, op0=ALU.add))
                dv(lambda e: e.reciprocal(out=s(14), in_=s(14)))
                dv(lambda e: e.tensor_tensor(out=GATE[:, tg, 0:1], in0=s(10), in1=s(14), op=ALU.mult), extra=["GATE"])
                dv(lambda e: e.tensor_tensor(out=GATE[:, tg, 1:2], in0=GATE[:, tg, 0:1], in1=s(13), op=ALU.mult), extra=["GATE"])
                dv(lambda e: e.tensor_tensor(out=A1.rearrange("p (g x) -> p g x", g=8), in0=goh.unsqueeze(2).to_broadcast([128, 8, 8]),
                                             in1=oh1.unsqueeze(1).to_broadcast([128, 8, 8]), op=ALU.mult))
                dv(lambda e: e.tensor_tensor(out=A2.rearrange("p (g x) -> p g x", g=8), in0=goh.unsqueeze(2).to_broadcast([128, 8, 8]),
                                             in1=oh2.unsqueeze(1).to_broadcast([128, 8, 8]), op=ALU.mult))
                dv(lambda e: e.tensor_tensor(out=Asum, in0=A1, in1=A2, op=ALU.add))
                P.op("pe", lambda e: e.matmul(ps[7][:, 0:64], lhsT=triS[:, :], rhs=Asum, start=True, stop=True), reads=RT + ["triS"], writes=["ps7"])
                P.op("pe", lambda e: e.matmul(ps[7][:, 64:128], lhsT=onesf[:, :], rhs=Asum, start=True, stop=True), reads=RT + ["onesf"], acc=["ps7"])
                dv(lambda e: e.tensor_tensor(out=pos, in0=ps[7][:, 0:64], in1=basebc[:, :], op=ALU.add), extra=["ps7", "basebc"])
                P.op("dve", lambda e: e.tensor_tensor(out=basebc[:, :], in0=basebc[:, :], in1=ps[7][:, 64:128], op=ALU.add), reads=["ps7", "basebc"] + RT, writes=["basebc"])
                dv(lambda e: e.tensor_tensor(out=pos, in0=pos, in1=eoff[:, :], op=ALU.add), extra=["eoff"])
                for kk, Ak in ((0, A1), (1, A2)):
                    dv(lambda e, Ak=Ak: e.tensor_tensor(out=tmp, in0=Ak, in1=pos, op=ALU.mult))
                    dv(lambda e, kk=kk: e.reduce_sum(out=s(16 + kk), in_=tmp, axis=AX.X))
                    dv(lambda e, kk=kk: e.tensor_copy(out=IDX[:, tg, kk:kk + 1], in_=s(16 + kk)), extra=["IDX"])
                    P.dma("pool", f"scat{kk}", lambda e, kk=kk: e.indirect_dma_start(
                        out=xb_d[:, :], out_offset=bass.IndirectOffsetOnAxis(ap=IDX[:, tg, kk:kk + 1], axis=0), in_=xn[:, :], in_offset=None,
                        bounds_check=NE * CAP - 1, oob_is_err=False), reads=["xn", "IDX", "xbz"], acc=["xbuf"])
                if debug:
                    dv(lambda e: e.tensor_copy(out=rt[:, 8, 0:1], in_=s(16)))
                    dv(lambda e: e.tensor_copy(out=rt[:, 8, 1:2], in_=s(17)))
                    dv(lambda e: e.tensor_copy(out=rt[:, 8, 2:4], in_=GATE[:, tg, :]))
                    P.dma("sp", "dbgrt", lambda e: e.dma_start(out=dbg["route"][:, tg, :], in_=rt[:, 8, 0:4]), reads=RT, acc=["dbgrt"])

            if do_moe:
                P.op("dve", lambda e: e.memset(mixT[:, 0:4, :], 0.0), writes=["mixT"])
                for e_ in range(NE):
                    P.dma("sp", "xbz", lambda e, e_=e_: e.dma_start(out=xb_d[e_ * CAP:(e_ + 1) * CAP, :], in_=mixT[:, 0:4, :].rearrange("p a b -> p (a b)")),
                          reads=["mixT"], acc=["xbz"])

            mixer_block(-1)
            for blk in range(nblk):
                mixer_block(blk)
                if debug and blk == 0:
                    for nm, src in (("ssdT", ssdT), ("lruT", lruT), ("mixT", mixT)):
                        for k in range(16):
                            P.op("act", lambda e, k=k, src=src: e.activation(out=tl[0][:, :], in_=src[:, k, :], func=AF.Copy), reads=[nm], writes=["tl0"])
                            P.dma("sp", "dbg" + nm, lambda e, k=k, nm=nm: e.dma_start(out=dbg[nm][:, k, :], in_=tl[0][:, :]), reads=["tl0"], acc=["dbg" + nm])
            P.barrier()

        if do_moe:
            with ExitStack() as st3:
                sb3 = lambda name, shape, dt: st3.enter_context(nc.sbuf_tensor("s_" + name, shape, dt))
                ring3 = Ring(P, [sb3(f"esl{i}", [128, 8192], BF16) for i in range(4)], "ew")
                Xe = [sb3(f"Xe{i}", [128, D], BF16) for i in range(2)]
                XeT = sb3("XeT", [128, 16, 128], BF16)
                sa = sb3("sa", [128, 512], F32)
                hb = sb3("hb", [128, 512], BF16)
                hT = sb3("hT", [128, 4, 128], BF16)
                ysb = [sb3(f"ysb{i}", [128, D], F32) for i in range(2)]
                gfin = sb3("gfin", [128, D], F32)
                hh = [sb3(f"hh{i}", [128, D], F32) for i in range(2)]
                y1 = [sb3(f"y1_{i}", [128, D], F32) for i in range(2)]
                y2 = [sb3(f"y2_{i}", [128, D], F32) for i in range(2)]
                sm3 = sb3("sm3", [128, 8], F32)
                P.dma("sp", "c_gfin", lambda e: e.dma_start(out=gfin[:], in_=gfin_d), writes=["gfin"])
                jobs = []

                def f_e(e_, fb, kind):
                    xi = e_ % 2

                    def fn(view, wres):
                        if kind == "w1":
                            if fb == 0:
                                P.dma("sp", f"Xe{xi}", lambda e: e.dma_start(out=Xe[xi][:, :], in_=xb_d[e_ * CAP:(e_ + 1) * CAP, :]), reads=["xbuf", "xbz"], writes=[f"Xe{xi}"])
                                for half in range(2):
                                    for j in range(8):
                                        k = half * 8 + j
                                        P.op("pe", lambda e, k=k, j=j: e.transpose(out=ps0[:, j * 128:(j + 1) * 128], in_=Xe[xi][:, k * 128:(k + 1) * 128], identity=identb[:, :]),
                                             reads=[f"Xe{xi}", "identb"], writes=["ps0"] if j == 0 else [], acc=["ps0"] if j else [], sig=(j == 7))
                                    P.op("dve", lambda e, half=half: e.tensor_copy(out=XeT[:, half * 8:half * 8 + 8, :], in_=ps0[:, :].rearrange("p (j t) -> p j t", j=8)),
                                         reads=["ps0"], writes=["XeT"] if half == 0 else [], acc=["XeT"] if half else [])
                        if kind in ("w1", "w3"):
                            bank = 5 if kind == "w1" else 6
                            for k in range(16):
                                P.op("pe", lambda e, k=k: e.matmul(ps[bank][:, :], lhsT=XeT[:, k, :], rhs=view[:, k, :], start=(k == 0), stop=(k == 15)),
                                     reads=[wres, "XeT"], writes=[f"ps{bank}"] if k == 0 else [], acc=[f"ps{bank}"] if k else [], sig=(k == 15))
                            if kind == "w1":
                                P.op("act", lambda e: e.activation(out=sa[:, :], in_=ps[5][:, :], func=AF.Silu), reads=["ps5"], writes=["sa"])
                            else:
                                P.op("dve", lambda e: e.tensor_tensor(out=hb[:, :], in0=sa[:, :], in1=ps[6][:, :], op=ALU.mult), reads=["sa", "ps6"], writes=["hb"])
                                for j in range(4):
                                    P.op("pe", lambda e, j=j: e.transpose(out=ps0[:, j * 128:(j + 1) * 128], in_=hb[:, j * 128:(j + 1) * 128], identity=identb[:, :]),
                                         reads=["hb", "identb"], writes=["ps0"] if j == 0 else [], acc=["ps0"] if j else [], sig=(j == 3))
                                P.op("dve", lambda e: e.tensor_copy(out=hT[:, :, :], in_=ps0[:, 0:512].rearrange("p (j t) -> p j t", j=4)), reads=["ps0"], writes=["hT"])
                            return
                        for d4 in range(4):
                            bank = 1 + d4
                            for ffc in range(4):
                                first = (fb == 0 and ffc == 0)
                                last = (fb == 1 and ffc == 3)
                                P.op("pe", lambda e, ffc=ffc, d4=d4, bank=bank, first=first, last=last: e.matmul(
                                    ps[bank][:, :], lhsT=hT[:, ffc, :], rhs=view[:, ffc, d4 * 512:(d4 + 1) * 512], start=first, stop=last),
                                    reads=[wres, "hT"], writes=[f"ps{bank}"] if first else [], acc=[] if first else [f"ps{bank}"], sig=(ffc == 3))
                        if fb == 1:
                            yi = e_ % 2
                            for d4 in range(4):
                                if d4 % 2 == 0:
                                    P.op("act", lambda e, d4=d4: e.activation(out=ysb[yi][:, d4 * 512:(d4 + 1) * 512], in_=ps[1 + d4][:, :], func=AF.Copy),
                                         reads=[f"ps{1 + d4}"], acc=[f"ysb{yi}"])
                                else:
                                    P.op("dve", lambda e, d4=d4: e.tensor_copy(out=ysb[yi][:, d4 * 512:(d4 + 1) * 512], in_=ps[1 + d4][:, :]),
                                         reads=[f"ps{1 + d4}"], acc=[f"ysb{yi}"])
                            P.dma("sp", f"yst{yi}", lambda e: e.dma_start(out=yb_d[e_ * CAP:(e_ + 1) * CAP, :], in_=ysb[yi][:, :]), reads=[f"ysb{yi}"], acc=["ybuf"])
                    return fn

                for e_ in range(NE):
                    for fb in range(2):
                        jobs.append(([(0, 16, w1_d[e_].rearrange("(k p) f -> p k f", p=128)[:, :, fb * 512:(fb + 1) * 512])], 16, 512, f_e(e_, fb, "w1")))
                        jobs.append(([(0, 16, w3_d[e_].rearrange("(k p) f -> p k f", p=128)[:, :, fb * 512:(fb + 1) * 512])], 16, 512, f_e(e_, fb, "w3")))
                        jobs.append(([(0, 4, w2_d[e_][fb * 512:(fb + 1) * 512, :].rearrange("(k p) n -> p k n", p=128))], 4, 2048, f_e(e_, fb, "w2")))
                run_jobs(ring3, jobs)

                for tg in range(16):
                    b = tg % 2
                    P.dma("sp", f"hh{b}", lambda e, b=b, tg=tg: e.dma_start(out=hh[b][:, :], in_=h1_d[tg * 128:(tg + 1) * 128, :]), reads=["h1d"], writes=[f"hh{b}"])
                    for kk, yy in ((0, y1), (1, y2)):
                        P.dma("pool", f"g{kk}_{b}", lambda e, kk=kk, yy=yy, b=b, tg=tg: e.indirect_dma_start(
                            out=yy[b][:, :], out_offset=None, in_=yb_d[:, :], in_offset=bass.IndirectOffsetOnAxis(ap=IDX[:, tg, kk:kk + 1], axis=0)),
                            reads=["ybuf", "IDX"], writes=[f"y{kk}_{b}"])
                        P.op("dve", lambda e, kk=kk, yy=yy, b=b, tg=tg: e.scalar_tensor_tensor(out=hh[b][:, :], in0=yy[b][:, :], scalar=GATE[:, tg, kk:kk + 1],
                                                                                               in1=hh[b][:, :], op0=ALU.mult, op1=ALU.add),
                             reads=[f"y{kk}_{b}", "GATE", f"hh{b}"], writes=[f"hh{b}"])
                    P.op("act", lambda e, b=b: e.activation(out=y1[b][:, :], in_=hh[b][:, :], func=AF.Square, accum_out=sm3[:, 0:1]), reads=[f"hh{b}"], writes=[f"y0_{b}", "sm3"])
                    P.op("dve", lambda e: e.tensor_scalar(out=sm3[:, 0:1], in0=sm3[:, 0:1], scalar1=1.0 / D, scalar2=EPS, op0=ALU.mult, op1=ALU.add), reads=["sm3"], writes=["sm3"])
                    P.op("act", lambda e: e.activation(out=sm3[:, 0:1], in_=sm3[:, 0:1], func=AF.Sqrt), reads=["sm3"], writes=["sm3"])
                    P.op("dve", lambda e: e.reciprocal(out=sm3[:, 0:1], in_=sm3[:, 0:1]), reads=["sm3"], writes=["sm3"])
                    P.op("dve", lambda e, b=b: e.scalar_tensor_tensor(out=y2[b][:, :], in0=hh[b][:, :], scalar=sm3[:, 0:1], in1=gfin[:, :], op0=ALU.mult, op1=ALU.mult),
                         reads=[f"hh{b}", "sm3", "gfin"], writes=[f"y1_{b}"])
                    P.dma("sp", f"ost{b}", lambda e, b=b, tg=tg: e.dma_start(out=out_d[tg * 128:(tg + 1) * 128, :], in_=y2[b][:, :]), reads=[f"y1_{b}"], acc=["outd"])
                P.wait_all("sp", ["outd"])
        else:
            P.wait_all("sp", ["h1d"])
        if debug:
            P.wait_all("sp", ["dbgh1", "dbgssdT", "dbglruT", "dbgmixT"] + (["dbgrt"] if do_moe else []))
        P.emit()
    return nc


def _pack_params(inp):
    pv = np.zeros((128, NPC), np.float32)

    def put(name, vec, col=0):
        v = np.asarray(vec, np.float32).reshape(-1, 128).T
        pv[:, PC[name] + col:PC[name] + col + v.shape[1]] = v
    put("gmix", inp["norm_mix"][0])
    for k in range(4):
        put("scw", inp["ssd_conv_w"][0, k], k * 32)
        put("lcw", inp["lru_conv_w"][0, k], k * 16)
    put("scb", inp["ssd_conv_b"][0])
    put("snw", inp["ssd_norm"][0])
    put("sd", np.repeat(np.asarray(inp["ssd_d"][0], np.float32), 64))
    put("lcb", inp["lru_conv_b"][0])
    put("lba", inp["lru_ba"][0])
    put("lbx", inp["lru_bx"][0])
    put("llam", inp["lru_lambda"][0])
    put("gb0", inp["gate_bias"][0, 0])
    put("gb1", inp["gate_bias"][0, 1])
    pv[0:32, PC["dtb"]] = np.asarray(inp["ssd_dt_bias"][0], np.float32)
    return pv


def make_in_maps(inp, cores):
    f = lambda a: np.ascontiguousarray(np.asarray(a, np.float32))
    shared = {
        "meta": np.ascontiguousarray(np.concatenate([np.zeros((128 - NMETA, D), np.float32), f(inp["meta_tokens"])], axis=0)),
        "pvec": _pack_params(inp),
        "alog_bc": np.ascontiguousarray(np.broadcast_to(np.asarray(inp["ssd_a_log"][0], np.float32)[None, :], (128, 32))),
        "gffn_bc": np.ascontiguousarray(np.broadcast_to(np.asarray(inp["norm_ffn"][0], np.float32)[None, :], (128, D))),
        "gfin_bc": np.ascontiguousarray(np.broadcast_to(np.asarray(inp["norm_final"], np.float32)[None, :], (128, D))),
        "wr": np.ascontiguousarray(np.concatenate([np.asarray(inp["w_router_group"][0], np.float32), np.asarray(inp["w_router_expert"][0], np.float32)], axis=1)),
        "w_in": f(inp["w_in"][0]),
        "w_ssd_out": f(inp["w_ssd_out"][0]),
        "w_lru_out": f(inp["w_lru_out"][0]),
        "w_out": f(inp["w_out"][0]),
        "lru_wa": f(inp["lru_wa"][0]),
        "lru_wx": f(inp["lru_wx"][0]),
        "w1": f(inp["w_exp_gate"][0]),
        "w3": f(inp["w_exp_up"][0]),
        "w2": f(inp["w_exp_down"][0]),
    }
    maps = []
    for c in cores:
        m = dict(shared)
        m["x"] = f(inp["x"][c])
        maps.append(m)
    return maps


def kernel(**inputs):
    nc = build()
    cores = list(range(8))
    in_maps = make_in_maps(inputs, cores)
    res = run_bass_kernel_spmd(nc, in_maps, core_ids=cores)
    return np.stack([np.asarray(r["out"], np.float32) for r in res.results], axis=0)
```

```python
import os
import numpy as np
from contextlib import ExitStack
import concourse.bass as bass
import concourse.mybir as mybir
from concourse.bass_utils import run_bass_kernel_spmd

F32 = mybir.dt.float32
BF16 = mybir.dt.bfloat16
I32 = mybir.dt.int32
ALU = mybir.AluOpType
AF = mybir.ActivationFunctionType
AX = mybir.AxisListType

ENGS = ("pe", "act", "dve", "pool", "sp")


class Prog:
    def __init__(self, nc, stack):
        self.nc = nc
        self.stack = stack
        self.ops = {e: [] for e in ENGS}
        self.cnt = {e: 0 for e in ENGS}
        self.seen = {e: {} for e in ENGS}
        self.sem = {e: stack.enter_context(nc.semaphore("sem_" + e)) for e in ENGS}
        self.w = {}
        self.r = {}
        self.dsem = {}
        self.unsig = {e: False for e in ENGS}
        self.alias = {"ps5": ("ps5", "ps5a", "ps5b")}

    def _exp(self, names):
        out = []
        for n in names:
            out.extend(self.alias.get(n, (n,)))
        return out

    def dma_sem(self, name):
        if name not in self.dsem:
            self.dsem[name] = [self.stack.enter_context(self.nc.semaphore("d_" + name)), 0]
        return self.dsem[name]

    def _deps(self, eng, reads, writes, acc):
        toks = []
        for res in reads:
            toks += self.w.get(res, [])
        for res in writes:
            toks += self.w.get(res, [])
            toks += self.r.get(res, [])
        for res in acc:
            toks += self.r.get(res, [])
        seen = self.seen[eng]
        best = {}
        for t in toks:
            key = (t[0], t[1])
            if t[2] <= seen.get(key, 0):
                continue
            if t[2] > best.get(key, 0):
                best[key] = t[2]
        waits = []
        for key, v in best.items():
            seen[key] = v
            waits.append((key, v))
        return waits

    def _commit(self, tok, reads, writes, acc):
        for res in reads:
            self.r.setdefault(res, []).append(tok)
        for res in writes:
            self.w[res] = [tok]
            self.r[res] = []
        for res in acc:
            self.w.setdefault(res, []).append(tok)

    def op(self, eng, fn, reads=(), writes=(), acc=(), sig=True):
        reads, writes, acc = self._exp(reads), self._exp(writes), self._exp(acc)
        waits = self._deps(eng, reads, writes, acc)
        n = self.cnt[eng] + 1
        if sig:
            self.cnt[eng] = n
        self.unsig[eng] = not sig
        self._commit(("e", eng, n), reads, writes, acc)
        self.ops[eng].append((waits, fn, sig, None))

    def dma(self, eng, semname, fn, reads=(), writes=(), acc=()):
        reads, writes, acc = self._exp(reads), self._exp(writes), self._exp(acc)
        waits = self._deps(eng, reads, writes, acc)
        s = self.dma_sem(semname)
        s[1] += 16
        self._commit(("d", semname, s[1]), reads, writes, acc)
        self.ops[eng].append((waits, fn, False, s[0]))

    def wait_all(self, eng, resources):
        waits = self._deps(eng, resources, (), ())
        self.ops[eng].append((waits, None, False, None))

    def barrier(self):
        for e in ENGS:
            assert not self.unsig[e], e
        for e in ENGS:
            waits = []
            seen = self.seen[e]
            for f in ENGS:
                if f != e and self.cnt[f] > seen.get(("e", f), 0):
                    seen[("e", f)] = self.cnt[f]
                    waits.append((("e", f), self.cnt[f]))
            for name, (s, v) in self.dsem.items():
                if v > seen.get(("d", name), 0):
                    seen[("d", name)] = v
                    waits.append((("d", name), v))
            self.ops[e].append((waits, None, False, None))

    def emit(self):
        nc = self.nc
        with nc.Block() as block:
            def run(eng_name):
                def body(e):
                    for waits, fn, sig, dsem in self.ops[eng_name]:
                        for key, v in waits:
                            if key[0] == "e":
                                e.wait_ge(self.sem[key[1]], v)
                            else:
                                e.wait_ge(self.dsem[key[1]][0], v)
                        if fn is None:
                            continue
                        ins = fn(e)
                        if dsem is not None:
                            ins.then_inc(dsem, 16)
                        elif sig:
                            ins.then_inc(self.sem[eng_name], 1)
                return body
            block.tensor(run("pe"))
            block.scalar(run("act"))
            block.vector(run("dve"))
            block.gpsimd(run("pool"))
            block.sync(run("sp"))


D = 2048
NMETA = 16
SEQ = 2048
TB = 512
NBLK = SEQ // TB
COL_Z, COL_XS, COL_B, COL_C, COL_DT, COL_LX, COL_LY, COL_G0, COL_G1, INC = 0, 2048, 4096, 5120, 6144, 6176, 8224, 10272, 12320, 14368
NE = 64
FF = 1024
CAP = 128
EPS = 1e-6

PC = {}
_off = 0
for _n, _c in (("gmix", 16), ("scw", 128), ("scb", 32), ("snw", 16), ("sd", 16), ("lcw", 64), ("lcb", 16), ("lba", 16),
               ("lbx", 16), ("llam", 16), ("gb0", 16), ("gb1", 16), ("dtb", 1)):
    PC[_n] = _off
    _off += _c
NPC = _off


class Ring:
    def __init__(self, P, slots, prefix):
        self.P, self.slots, self.prefix, self.n = P, slots, prefix, 0

    def load(self, srcs, k, n):
        i = self.n % len(self.slots)
        self.n += 1
        view = self.slots[i][:, 0:k * n].rearrange("p (k n) -> p k n", k=k)
        res = f"{self.prefix}{i}"
        for q, (k0, k1, src) in enumerate(srcs):
            self.P.dma("pool", res, lambda e, k0=k0, k1=k1, src=src: e.dma_start(out=view[:, k0:k1, :], in_=src),
                       writes=[res] if q == 0 else [], acc=[res] if q else [])
        return view, res


def run_jobs(ring, jobs):
    nw = len(ring.slots)
    wj = [j for j in jobs if j[0] is not None]
    loaded = {}
    nxt = 0
    wi = 0
    for j in jobs:
        if j[0] is not None:
            while nxt < len(wj) and nxt <= wi + nw - 1:
                jj = wj[nxt]
                loaded[id(jj)] = ring.load(jj[0], jj[1], jj[2])
                nxt += 1
            v, r = loaded.pop(id(j))
            wi += 1
            j[3](v, r)
        else:
            j[3](None, None)


def build(debug=False, nblk=NBLK, do_moe=True):
    nc = bass.Bass("TRN2", target_bir_lowering=False)
    dt_ = nc.dram_tensor
    x_d = dt_("x", [SEQ, D], F32, kind="ExternalInput").ap()
    meta_d = dt_("meta", [128, D], F32, kind="ExternalInput").ap()
    pvec_d = dt_("pvec", [128, NPC], F32, kind="ExternalInput").ap()
    alog_d = dt_("alog_bc", [128, 32], F32, kind="ExternalInput").ap()
    gffn_d = dt_("gffn_bc", [128, D], F32, kind="ExternalInput").ap()
    gfin_d = dt_("gfin_bc", [128, D], F32, kind="ExternalInput").ap()
    wr_d = dt_("wr", [D, 72], F32, kind="ExternalInput").ap()
    win_d = dt_("w_in", [D, INC], F32, kind="ExternalInput").ap()
    wso_d = dt_("w_ssd_out", [D, D], F32, kind="ExternalInput").ap()
    wlo_d = dt_("w_lru_out", [D, D], F32, kind="ExternalInput").ap()
    wo_d = dt_("w_out", [D, D], F32, kind="ExternalInput").ap()
    wa_d = dt_("lru_wa", [8, 256, 256], F32, kind="ExternalInput").ap()
    wx_d = dt_("lru_wx", [8, 256, 256], F32, kind="ExternalInput").ap()
    if do_moe:
        w1_d = dt_("w1", [NE, D, FF], F32, kind="ExternalInput").ap()
        w3_d = dt_("w3", [NE, D, FF], F32, kind="ExternalInput").ap()
        w2_d = dt_("w2", [NE, FF, D], F32, kind="ExternalInput").ap()
    out_d = dt_("out", [SEQ, D], F32, kind="ExternalOutput").ap()
    h1_d = dt_("h1s", [SEQ, D], F32, kind="Internal").ap()
    xb_d = dt_("xbuf", [NE * CAP, D], BF16, kind="Internal").ap()
    yb_d = dt_("ybuf", [NE * CAP, D], F32, kind="Internal").ap()
    dbg = {}
    if debug:
        dbg["ssdT"] = dt_("dbg_ssdT", [128, 16, TB], F32, kind="ExternalOutput").ap()
        dbg["lruT"] = dt_("dbg_lruT", [128, 16, TB], F32, kind="ExternalOutput").ap()
        dbg["mixT"] = dt_("dbg_mixT", [128, 16, TB], F32, kind="ExternalOutput").ap()
        dbg["h1"] = dt_("dbg_h1", [SEQ, D], F32, kind="ExternalOutput").ap()
        dbg["route"] = dt_("dbg_route", [128, 16, 4], F32, kind="ExternalOutput").ap()

    with ExitStack() as st:
        P = Prog(nc, st)
        sb = lambda name, shape, dt: st.enter_context(nc.sbuf_tensor("s_" + name, shape, dt))
        pv = sb("pv", [128, NPC], F32)
        alog = sb("alog", [128, 32], F32)
        Aneg = sb("Aneg", [128, 32], F32)
        klam = sb("klam", [128, 16], F32)
        identf = sb("identf", [128, 128], F32)
        identb = sb("identb", [128, 128], BF16)
        triU = sb("triU", [128, 128], F32)
        triS = sb("triS", [128, 128], F32)
        negm = sb("negm", [128, 128], F32)
        onesf = sb("onesf", [128, 128], F32)
        iof = sb("iof", [128, 128], F32)
        pidx = sb("pidx", [128, 1], F32)
        eoff = sb("eoff", [128, 64], F32)
        wr = sb("wr", [128, 16, 72], F32)
        gffn = sb("gffn", [128, D], F32)
        ctail = sb("ctail", [128, 48, 3], F32)
        hst = sb("hst", [128, 16], F32)
        ST = sb("ST", [128, 32, 64], F32)
        STb = sb("STb", [128, 32, 64], BF16)
        basebc = sb("basebc", [128, 64], F32)
        IDX = sb("IDX", [128, 16, 2], I32)
        GATE = sb("GATE", [128, 16, 2], F32)
        ps0 = st.enter_context(nc.psum_tensor("ps0", [128, 1024], BF16))
        ps = [None] + [st.enter_context(nc.psum_tensor(f"ps{i}", [128, 512], F32)) for i in range(1, 8)]

        P.dma("sp", "c_pv", lambda e: e.dma_start(out=pv[:], in_=pvec_d), writes=["pv"])
        P.dma("sp", "c_alog", lambda e: e.dma_start(out=alog[:], in_=alog_d), writes=["alog"])
        P.dma("sp", "c_gffn", lambda e: e.dma_start(out=gffn[:], in_=gffn_d), writes=["gffn"])
        P.dma("sp", "c_wr", lambda e: e.dma_start(out=wr[:], in_=wr_d.rearrange("(k p) n -> p k n", p=128)), writes=["wr"])
        P.op("pool", lambda e: e.iota(iof[:], pattern=[[1, 128]], base=0, channel_multiplier=0, allow_small_or_imprecise_dtypes=True), writes=["iof"])
        P.op("pool", lambda e: e.iota(pidx[:], pattern=[[0, 1]], base=0, channel_multiplier=1, allow_small_or_imprecise_dtypes=True), writes=["pidx"])
        P.op("dve", lambda e: e.tensor_scalar(out=identf[:], in0=iof[:], scalar1=pidx[:, 0:1], scalar2=None, op0=ALU.is_equal), reads=["iof", "pidx"], writes=["identf"])
        P.op("dve", lambda e: e.tensor_copy(out=identb[:], in_=identf[:]), reads=["identf"], writes=["identb"])
        P.op("dve", lambda e: e.tensor_scalar(out=triU[:], in0=iof[:], scalar1=pidx[:, 0:1], scalar2=None, op0=ALU.is_ge), reads=["iof", "pidx"], writes=["triU"])
        P.op("dve", lambda e: e.tensor_scalar(out=triS[:], in0=iof[:], scalar1=pidx[:, 0:1], scalar2=None, op0=ALU.is_gt), reads=["iof", "pidx"], writes=["triS"])
        P.op("dve", lambda e: e.tensor_scalar(out=negm[:], in0=triU[:], scalar1=-1.0, scalar2=1e30, op0=ALU.add, op1=ALU.mult), reads=["triU"], writes=["negm"])
        P.op("dve", lambda e: e.memset(onesf[:], 1.0), writes=["onesf"])
        Lgt = sb("Lgt", [128, 128], F32)
        P.op("dve", lambda e: e.tensor_scalar(out=Lgt[:], in0=iof[:], scalar1=pidx[:, 0:1], scalar2=None, op0=ALU.is_lt), reads=["iof", "pidx"], writes=["Lgt"])
        tmask = sb("tmask", [128, 128], F32)
        P.op("dve", lambda e: e.tensor_scalar(out=tmask[:], in0=iof[:], scalar1=float(128 - NMETA), scalar2=None, op0=ALU.is_ge), reads=["iof"], writes=["tmask"])
        P.op("dve", lambda e: e.tensor_scalar(out=eoff[:], in0=iof[:, 0:64], scalar1=float(CAP), scalar2=None, op0=ALU.mult), reads=["iof"], writes=["eoff"])
        P.op("dve", lambda e: e.memset(ctail[:], 0.0), writes=["ctail"])
        P.op("dve", lambda e: e.memset(hst[:], 0.0), writes=["hst"])
        P.op("dve", lambda e: e.memset(ST[:], 0.0), writes=["ST"])
        P.op("dve", lambda e: e.memset(STb[:], 0.0), writes=["STb"])
        P.op("dve", lambda e: e.memset(basebc[:], 0.0), writes=["basebc"])
        P.op("act", lambda e: e.activation(out=Aneg[:], in_=alog[:], func=AF.Exp), reads=["alog"], writes=["Aneg"])
        P.op("dve", lambda e: e.tensor_scalar(out=Aneg[:], in0=Aneg[:], scalar1=-1.0, scalar2=None, op0=ALU.mult), reads=["Aneg"], writes=["Aneg"])
        c_lam = PC["llam"]
        P.op("act", lambda e: e.activation(out=klam[:], in_=pv[:, c_lam:c_lam + 16], func=AF.Exp, scale=-1.0), reads=["pv"], writes=["klam"])
        P.op("act", lambda e: e.activation(out=klam[:], in_=klam[:], func=AF.Ln, bias=onesf[:, 0:1]), reads=["klam", "onesf"], writes=["klam"])
        P.op("dve", lambda e: e.tensor_scalar(out=klam[:], in0=klam[:], scalar1=-8.0, scalar2=None, op0=ALU.mult), reads=["klam"], writes=["klam"])

        WT = 256
        winv = win_d.rearrange("(k p) n -> p k n", p=128)

        def win_tile(c0, n=WT):
            return [(0, 16, winv[:, :, c0:c0 + n])]

        def sq_tile(w_d, c0):
            return [(0, 16, w_d.rearrange("(k p) n -> p k n", p=128)[:, :, c0:c0 + WT])]

        with ExitStack() as st2:
            sb2 = lambda name, shape, dt: st2.enter_context(nc.sbuf_tensor("s_" + name, shape, dt))
            ring = Ring(P, [sb2(f"wsl{i}", [128, 16 * WT], BF16) for i in range(3)], "w")
            uT = sb2("uT", [128, 16, TB], BF16)
            ssdT = sb2("ssdT", [128, 16, TB], BF16)
            lruT = sb2("lruT", [128, 16, TB], BF16)
            mixT = sb2("mixT", [128, 16, TB], BF16)
            xt = sb2("xt", [128, D], F32)
            xn = sb2("xn", [128, D], BF16)
            xsub = [sb2(f"xsub{i}", [128, WT], F32) for i in range(2)]
            sm = sb2("sm", [128, 32], F32)
            rt = sb2("rt", [128, 10, 72], F32)
            BTs = sb2("BTs", [128, 2, TB], BF16)
            CTs = sb2("CTs", [128, 2, TB], BF16)
            xsT = sb2("xsT", [128, 2, TB], BF16)
            yT = sb2("yT", [128, 2, TB], F32)
            dtT = sb2("dtT", [32, TB], F32)
            cbuf2 = [sb2(f"cbuf{i}", [128, TB + 3], F32) for i in range(2)]
            cacc2 = [sb2(f"cacc{i}", [128, TB], F32) for i in range(2)]
            xrb = sb2("xrb", [128, 2, TB], BF16)
            glb = sb2("glb", [128, 2, TB], BF16)
            Gs = sb2("Gs", [128, 2, TB], BF16)
            tl = [sb2(f"tl{i}", [128, TB], F32) for i in range(8)]
            u2T = tl[4][:, :].rearrange("p (k t) -> p k t", k=4)
            u2Tb = tl[5][:, :].rearrange("p (k t) -> p k t", k=4)
            dtk = sb2("dtk", [128, 4, 32], F32)
            adt = sb2("adt", [128, 4, 32], F32)
            acum = sb2("acum", [128, 4, 32], F32)
            dte = sb2("dte", [128, 4, 32], F32)
            eend = sb2("eend", [128, 4, 32], F32)
            xdt2 = [sb2(f"xdt{i}", [128, 256], BF16) for i in range(2)]
            xdtw2 = [sb2(f"xdtw{i}", [128, 256], BF16) for i in range(2)]
            Btok2 = [sb2(f"Btok{i}", [128, 128], BF16) for i in range(2)]
            Rm2 = [sb2(f"Rm{i}", [128, 512], F32) for i in range(2)]
            Ee2 = [sb2(f"Ee{i}", [128, 512], F32) for i in range(2)]
            MT2 = [sb2(f"MT{i}", [128, 512], BF16) for i in range(2)]
            eB2 = [sb2(f"eB{i}", [128, 512], F32) for i in range(2)]
            CdT2 = [sb2(f"CdT{i}", [128, 512], BF16) for i in range(2)]
            cbm2 = [sb2(f"cbm{i}", [128, 128], F32) for i in range(2)]
            cnt = {"conv": 0, "ssd": 0, "lru": 0}
            v4 = lambda ap: ap.rearrange("p (j l) -> p j l", j=4)

            def rms_rstd(src_ap, q, scratch_ap, scr_res, col, tag):
                P.op("act", lambda e: e.activation(out=scratch_ap, in_=src_ap, func=AF.Square, accum_out=sm[0:q, col:col + 1]),
                     reads=[tag], writes=[scr_res, "sm"])
                P.op("dve", lambda e: e.tensor_scalar(out=sm[0:q, col:col + 1], in0=sm[0:q, col:col + 1], scalar1=1.0 / D, scalar2=EPS,
                                                      op0=ALU.mult, op1=ALU.add), reads=["sm"], writes=["sm"])
                P.op("act", lambda e: e.activation(out=sm[0:q, col:col + 1], in_=sm[0:q, col:col + 1], func=AF.Sqrt), reads=["sm"], writes=["sm"])
                P.op("dve", lambda e: e.reciprocal(out=sm[0:q, col:col + 1], in_=sm[0:q, col:col + 1]), reads=["sm"], writes=["sm"])

            def mixer_block(blk):
                is_meta = blk < 0
                tb = 128 if is_meta else TB
                nq = 1 if is_meta else TB // 128
                Q = 128
                jobs = []

                def build_u(_v, _r):
                    for ti in range(nq):
                        src = meta_d if is_meta else x_d[blk * TB + ti * 128: blk * TB + (ti + 1) * 128, :]
                        P.dma("sp", "xt", lambda e, src=src: e.dma_start(out=xt[0:Q, :], in_=src), writes=["xt"])
                        rms_rstd(xt[0:Q, :], Q, xn[0:Q, :], "xn", 0, "xt")
                        P.op("act", lambda e: e.activation(out=xn[0:Q, :], in_=xt[0:Q, :], func=AF.Identity, scale=sm[0:Q, 0:1]),
                             reads=["xt", "sm"], writes=["xn"])
                        for half in range(2):
                            for j in range(8):
                                k = half * 8 + j
                                P.op("pe", lambda e, k=k, j=j: e.transpose(out=ps0[:, j * 128:j * 128 + Q], in_=xn[0:Q, k * 128:(k + 1) * 128],
                                                                       identity=identb[0:Q, 0:Q]),
                                     reads=["xn", "identb"], writes=["ps0"] if j == 0 else [], acc=["ps0"] if j else [], sig=(j == 7))
                            g0 = PC["gmix"] + half * 8
                            P.op("dve", lambda e, half=half, ti=ti, g0=g0: e.tensor_tensor(
                                out=uT[:, half * 8:half * 8 + 8, ti * 128:ti * 128 + Q],
                                in0=ps0[:, :].rearrange("p (j t) -> p j t", j=8)[:, :, 0:Q],
                                in1=pv[:, g0:g0 + 8].unsqueeze(2).to_broadcast([128, 8, Q]), op=ALU.mult),
                                reads=["ps0", "pv"], acc=["uT"])
                jobs.append((None, 0, 0, build_u))

                def proj(view, wres, j, ncol, bank):
                    bres = f"ps{bank}"
                    for k in range(16):
                        P.op("pe", lambda e, k=k: e.matmul(ps[bank][0:ncol, 0:tb], lhsT=view[:, k, j * 128:j * 128 + ncol], rhs=uT[:, k, 0:tb],
                                                           start=(k == 0), stop=(k == 15)),
                             reads=[wres, "uT"], writes=[bres] if k == 0 else [], acc=[bres] if k else [], sig=(k == 15))

                def conv_chunk(bank, tcol, wcol0, wstride, bcol, out_ap, out_res, func):
                    bres = f"ps{bank}"
                    pp = cnt["conv"] % 2
                    cnt["conv"] += 1
                    cbuf, cacc = cbuf2[pp], cacc2[pp]
                    cb_, ca_ = f"cbuf{pp}", f"cacc{pp}"
                    P.op("act", lambda e: e.activation(out=cbuf[:, 3:3 + tb], in_=ps[bank][:, 0:tb], func=AF.Copy), reads=[bres], writes=[cb_])
                    P.op("act", lambda e: e.activation(out=cbuf[:, 0:3], in_=ctail[:, tcol, :], func=AF.Copy), reads=["ctail"], acc=[cb_])
                    P.op("act", lambda e: e.activation(out=cacc[:, 0:tb], in_=cbuf[:, 0:tb], func=AF.Identity, scale=pv[:, wcol0:wcol0 + 1],
                                                       bias=pv[:, bcol:bcol + 1]),
                         reads=[cb_, "pv"], writes=[ca_])
                    for kk in (1, 2, 3):
                        last = (kk == 3 and func is None)
                        P.op("dve", lambda e, kk=kk, last=last: e.scalar_tensor_tensor(
                            out=(out_ap if last else cacc[:, 0:tb]), in0=cbuf[:, kk:kk + tb],
                            scalar=pv[:, wcol0 + kk * wstride:wcol0 + kk * wstride + 1], in1=cacc[:, 0:tb], op0=ALU.mult, op1=ALU.add),
                            reads=[cb_, ca_, "pv"], writes=[] if last else [ca_], acc=[out_res] if last else [])
                    if func is not None:
                        P.op("act", lambda e: e.activation(out=out_ap, in_=cacc[:, 0:tb], func=func), reads=[ca_], acc=[out_res])
                    P.op("act", lambda e: e.activation(out=ctail[:, tcol, :], in_=cbuf[:, tb:tb + 3], func=AF.Copy), reads=[cb_], writes=["ctail"])

                def f_ly(hd):
                    def fn(view, wres):
                        for jc in range(2):
                            bank = 1 + jc
                            proj(view, wres, jc, 128, bank)
                            P.op("act", lambda e, bank=bank, jc=jc: e.activation(out=glb[:, jc, 0:tb], in_=ps[bank][:, 0:tb], func=AF.Gelu_apprx_tanh),
                                 reads=[f"ps{bank}"], acc=["glb"])
                    return fn

                def f_lx(hd):
                    def fn(view, wres):
                        for ic in range(2):
                            c = hd * 2 + ic
                            bank = 1 + ic
                            proj(view, wres, ic, 128, bank)
                            conv_chunk(bank, 32 + c, PC["lcw"] + c, 16, PC["lcb"] + c, xrb[:, ic, 0:tb], "xrb", None)
                    return fn

                def f_wg(hd):
                    def fn(view, wres):
                        for jc in range(2):
                            c = hd * 2 + jc
                            sp_ = cnt["lru"] % 2
                            cnt["lru"] += 1
                            r_, ig_, a_, m_ = tl[sp_ * 4:sp_ * 4 + 4]
                            rn_, in_, an_, mn_ = [f"tl{sp_ * 4 + i}" for i in range(4)]
                            hs_, hn_ = r_, rn_
                            for gi, bank in ((0, 3), (1, 4)):
                                for ic in range(2):
                                    P.op("pe", lambda e, gi=gi, ic=ic, bank=bank, jc=jc: e.matmul(
                                        ps[bank][:, 0:tb], lhsT=view[:, gi * 2 + ic, jc * 128:(jc + 1) * 128], rhs=xrb[:, ic, 0:tb],
                                        start=(ic == 0), stop=(ic == 1)),
                                        reads=[wres, "xrb"], writes=[f"ps{bank}"] if ic == 0 else [], acc=[f"ps{bank}"] if ic else [], sig=(ic == 1))
                            P.op("act", lambda e, c=c, r_=r_: e.activation(out=r_[:, 0:tb], in_=ps[3][:, 0:tb], func=AF.Sigmoid, bias=pv[:, PC["lba"] + c:PC["lba"] + c + 1]),
                                 reads=["ps3", "pv"], writes=[rn_])
                            P.op("act", lambda e, c=c, ig_=ig_: e.activation(out=ig_[:, 0:tb], in_=ps[4][:, 0:tb], func=AF.Sigmoid, bias=pv[:, PC["lbx"] + c:PC["lbx"] + c + 1]),
                                 reads=["ps4", "pv"], writes=[in_])
                            P.op("act", lambda e, c=c, a_=a_, r_=r_: e.activation(out=a_[:, 0:tb], in_=r_[:, 0:tb], func=AF.Exp, scale=klam[:, c:c + 1]),
                                 reads=[rn_, "klam"], writes=[an_])
                            P.op("dve", lambda e, m_=m_, a_=a_: e.tensor_tensor(out=m_[:, 0:tb], in0=a_[:, 0:tb], in1=a_[:, 0:tb], op=ALU.mult), reads=[an_], writes=[mn_])
                            P.op("dve", lambda e, m_=m_: e.tensor_scalar(out=m_[:, 0:tb], in0=m_[:, 0:tb], scalar1=-1.0, scalar2=1.0, op0=ALU.mult, op1=ALU.add),
                                 reads=[mn_], writes=[mn_])
                            P.op("act", lambda e, m_=m_: e.activation(out=m_[:, 0:tb], in_=m_[:, 0:tb], func=AF.Sqrt), reads=[mn_], writes=[mn_])
                            P.op("dve", lambda e, m_=m_, ig_=ig_: e.tensor_tensor(out=m_[:, 0:tb], in0=m_[:, 0:tb], in1=ig_[:, 0:tb], op=ALU.mult), reads=[mn_, in_], writes=[mn_])
                            P.op("dve", lambda e, jc=jc, m_=m_: e.tensor_tensor(out=m_[:, 0:tb], in0=m_[:, 0:tb], in1=xrb[:, jc, 0:tb], op=ALU.mult), reads=[mn_, "xrb"], writes=[mn_])
                            if is_meta:
                                P.op("dve", lambda e, m_=m_: e.tensor_tensor(out=m_[:, 0:tb], in0=m_[:, 0:tb], in1=tmask[:, :], op=ALU.mult), reads=[mn_, "tmask"], writes=[mn_])
                            P.op("dve", lambda e, c=c, hs_=hs_, a_=a_, m_=m_: e.tensor_tensor_scan(out=hs_[:, 0:tb], data0=a_[:, 0:tb], data1=m_[:, 0:tb], initial=hst[:, c:c + 1],
                                                                                              op0=ALU.mult, op1=ALU.add), reads=[an_, mn_, "hst"], writes=[hn_])
                            P.op("act", lambda e, c=c, hs_=hs_: e.activation(out=hst[:, c:c + 1], in_=hs_[:, tb - 1:tb], func=AF.Copy), reads=[hn_], writes=["hst"])
                            if not is_meta:
                                P.op("dve", lambda e, c=c, jc=jc, hs_=hs_: e.tensor_tensor(out=lruT[:, c, 0:tb], in0=hs_[:, 0:tb], in1=glb[:, jc, 0:tb], op=ALU.mult),
                                     reads=[hn_, "glb"], acc=["lruT"])
                    return fn

                for hd in range(8):
                    if not is_meta:
                        jobs.append((win_tile(COL_LY + hd * 256), 16, WT, f_ly(hd)))
                    jobs.append((win_tile(COL_LX + hd * 256), 16, WT, f_lx(hd)))
                    wg_srcs = [(0, 2, wa_d[hd].rearrange("(ic p) j -> p ic j", p=128)), (2, 4, wx_d[hd].rearrange("(ic p) j -> p ic j", p=128))]
                    jobs.append((wg_srcs, 4, 256, f_wg(hd)))

                def f_dt(view, wres):
                    for k in range(16):
                        P.op("pe", lambda e, k=k: e.matmul(ps[5][0:32, 0:tb], lhsT=view[:, k, 0:32], rhs=uT[:, k, 0:tb], start=(k == 0), stop=(k == 15)),
                             reads=[wres, "uT"], writes=["ps5"] if k == 0 else [], acc=["ps5"] if k else [], sig=(k == 15))
                    P.op("act", lambda e: e.activation(out=dtT[:, 0:tb], in_=ps[5][0:32, 0:tb], func=AF.Exp, bias=pv[0:32, PC["dtb"]:PC["dtb"] + 1]),
                         reads=["ps5", "pv"], writes=["dtT"])
                    P.op("act", lambda e: e.activation(out=dtT[:, 0:tb], in_=dtT[:, 0:tb], func=AF.Ln, bias=onesf[0:32, 0:1]), reads=["dtT", "onesf"], writes=["dtT"])
                    if is_meta:
                        P.op("dve", lambda e: e.tensor_tensor(out=dtT[:, 0:tb], in0=dtT[:, 0:tb], in1=tmask[0:32, :], op=ALU.mult), reads=["dtT", "tmask"], writes=["dtT"])
                    for q in range(nq):
                        P.op("pe", lambda e, q=q: e.transpose(out=ps[5][0:Q, 0:32], in_=dtT[:, q * 128:q * 128 + Q], identity=identf[0:32, 0:32]),
                             reads=["dtT", "identf"], writes=["ps5"])
                        P.op("act", lambda e, q=q: e.activation(out=dtk[0:Q, q, :], in_=ps[5][0:Q, 0:32], func=AF.Copy), reads=["ps5"], acc=["dtk"])
                        P.op("dve", lambda e, q=q: e.tensor_tensor(out=adt[0:Q, q, :], in0=ps[5][0:Q, 0:32], in1=Aneg[0:Q, :], op=ALU.mult),
                             reads=["ps5", "Aneg"], acc=["adt"])
                        P.op("pe", lambda e, q=q: e.matmul(ps[6][0:Q, 0:32], lhsT=triU[0:Q, 0:Q], rhs=adt[0:Q, q, :], start=True, stop=True),
                             reads=["adt", "triU"], writes=["ps6"])
                        P.op("pe", lambda e, q=q: e.matmul(ps[6][:, 32:64], lhsT=onesf[0:Q, :], rhs=adt[0:Q, q, :], start=True, stop=True),
                             reads=["adt", "onesf"], acc=["ps6"])
                        P.op("act", lambda e, q=q: e.activation(out=acum[0:Q, q, :], in_=ps[6][0:Q, 0:32], func=AF.Copy), reads=["ps6"], acc=["acum"])
                        P.op("dve", lambda e, q=q: e.tensor_tensor(out=dte[0:Q, q, :], in0=ps[6][0:Q, 32:64], in1=acum[0:Q, q, :], op=ALU.subtract),
                             reads=["ps6", "acum"], acc=["dte"])
                        P.op("act", lambda e, q=q: e.activation(out=dte[0:Q, q, :], in_=dte[0:Q, q, :], func=AF.Exp), reads=["dte"], acc=["dte"])
                        P.op("act", lambda e, q=q: e.activation(out=eend[:, q, :], in_=ps[6][:, 32:64], func=AF.Exp), reads=["ps6"], acc=["eend"])
                jobs.append((win_tile(COL_DT, 32), 16, 32, f_dt))

                def f_bc(which, gp):
                    dst = BTs if which == 0 else CTs
                    dres = "BTs" if which == 0 else "CTs"

                    def fn(view, wres):
                        for j in range(2):
                            c = (16 if which == 0 else 24) + gp * 2 + j
                            bank = 1 + j
                            proj(view, wres, j, 128, bank)
                            conv_chunk(bank, c, PC["scw"] + c, 32, PC["scb"] + c, dst[:, j, 0:tb], dres, AF.Silu)
                    return fn

                def f_xs(g):
                    def fn(view, wres):
                        for j in range(2):
                            c = g * 2 + j
                            bank = 1 + j
                            proj(view, wres, j, 128, bank)
                            conv_chunk(bank, c, PC["scw"] + c, 32, PC["scb"] + c, xsT[:, j, 0:tb], "xsT", AF.Silu)
                    return fn

                def ssd_group(g, gl_, zview, zres):
                    for q in range(nq):
                        tsl = slice(q * 128, q * 128 + Q)
                        pp = cnt["ssd"] % 2
                        cnt["ssd"] += 1
                        xdt, xdtw, Btok, Rm, Ee, MT, eB, CdT, cbm = xdt2[pp], xdtw2[pp], Btok2[pp], Rm2[pp], Ee2[pp], MT2[pp], eB2[pp], CdT2[pp], cbm2[pp]
                        n_ = lambda nm: f"{nm}{pp}"
                        for jj in range(2):
                            P.op("pe", lambda e, tsl=tsl, jj=jj: e.transpose(out=ps0[0:Q, jj * 128:(jj + 1) * 128], in_=xsT[:, jj, tsl], identity=identb[:, :]),
                                 reads=["xsT", "identb"], writes=["ps0"] if jj == 0 else [], acc=["ps0"] if jj else [], sig=False)
                        P.op("pe", lambda e, tsl=tsl: e.transpose(out=ps0[0:Q, 256:384], in_=BTs[:, gl_, tsl], identity=identb[:, :]),
                             reads=["BTs", "identb"], acc=["ps0"])
                        P.op("dve", lambda e, q=q, xdt=xdt: e.tensor_tensor(out=xdt[0:Q, :].rearrange("p (h d) -> p h d", h=4),
                                                                            in0=ps0[0:Q, 0:256].rearrange("p (h d) -> p h d", h=4),
                                                                            in1=dtk[0:Q, q, 4 * g:4 * g + 4].unsqueeze(2).to_broadcast([Q, 4, 64]), op=ALU.mult),
                             reads=["ps0", "dtk"], writes=[n_("xdt")])
                        P.op("dve", lambda e, q=q, xdt=xdt, xdtw=xdtw: e.tensor_tensor(out=xdtw[0:Q, :].rearrange("p (h d) -> p h d", h=4),
                                                                                       in0=xdt[0:Q, :].rearrange("p (h d) -> p h d", h=4),
                                                                                       in1=dte[0:Q, q, 4 * g:4 * g + 4].unsqueeze(2).to_broadcast([Q, 4, 64]), op=ALU.mult),
                             reads=[n_("xdt"), "dte"], writes=[n_("xdtw")])
                        P.op("dve", lambda e, Btok=Btok: e.tensor_copy(out=Btok[0:Q, :], in_=ps0[0:Q, 256:384]), reads=["ps0"], writes=[n_("Btok")])
                        if not is_meta:
                            P.op("pe", lambda e, tsl=tsl: e.matmul(ps[5][:, 0:128], lhsT=BTs[:, gl_, tsl], rhs=CTs[:, gl_, tsl], start=True, stop=True),
                                 reads=["BTs", "CTs"], writes=["ps5a"])
                            P.op("dve", lambda e, cbm=cbm: e.tensor_tensor(out=cbm[:, :], in0=ps[5][:, 0:128], in1=triU[:, :], op=ALU.mult),
                                 reads=["ps5a", "triU"], writes=[n_("cbm")])
                            P.op("dve", lambda e, q=q, Rm=Rm: e.tensor_tensor(out=v4(Rm[:, :]),
                                                                              in0=adt[:, q, 4 * g:4 * g + 4].unsqueeze(2).to_broadcast([128, 4, 128]),
                                                                              in1=triU[:, :].unsqueeze(1).to_broadcast([128, 4, 128]), op=ALU.mult),
                                 reads=["adt", "triU"], writes=[n_("Rm")])
                            P.op("pe", lambda e, Rm=Rm: e.matmul(ps[6][:, :], lhsT=Lgt[:, :], rhs=Rm[:, :], start=True, stop=True), reads=[n_("Rm"), "Lgt"], writes=["ps6"])
                            P.op("pe", lambda e, Rm=Rm: e.matmul(ps[7][:, :], lhsT=onesf[:, :], rhs=Rm[:, :], start=True, stop=True), reads=[n_("Rm"), "onesf"], writes=["ps7"])
                            P.op("act", lambda e, Ee=Ee: e.activation(out=Ee[:, :], in_=ps[6][:, :], func=AF.Exp), reads=["ps6"], writes=[n_("Ee")])
                            P.op("act", lambda e, eB=eB: e.activation(out=eB[:, :], in_=ps[7][:, :], func=AF.Exp), reads=["ps7"], writes=[n_("eB")])
                            P.op("dve", lambda e, MT=MT, Ee=Ee, cbm=cbm: e.tensor_tensor(out=v4(MT[:, :]), in0=v4(Ee[:, :]),
                                                                                         in1=cbm[:, :].unsqueeze(1).to_broadcast([128, 4, 128]), op=ALU.mult),
                                 reads=[n_("Ee"), n_("cbm")], writes=[n_("MT")])
                            P.op("dve", lambda e, tsl=tsl, CdT=CdT, eB=eB: e.tensor_tensor(out=v4(CdT[:, :]), in0=v4(eB[:, :]),
                                                                                           in1=CTs[:, gl_, tsl].unsqueeze(1).to_broadcast([128, 4, 128]), op=ALU.mult),
                                 reads=[n_("eB"), "CTs"], writes=[n_("CdT")])
                            for j in range(4):
                                h = 4 * g + j
                                po = (j % 2) * 64
                                bank = 3 + j // 2
                                first = (j % 2 == 0)
                                P.op("pe", lambda e, j=j, po=po, bank=bank, xdt=xdt, MT=MT: e.matmul(ps[bank][po:po + 64, 0:128], lhsT=xdt[:, j * 64:(j + 1) * 64],
                                                                                                      rhs=MT[:, j * 128:(j + 1) * 128], start=True, stop=False),
                                     reads=[n_("xdt"), n_("MT")], writes=[f"ps{bank}"] if first else [], acc=[] if first else [f"ps{bank}"], sig=False)
                                P.op("pe", lambda e, j=j, po=po, bank=bank, h=h, CdT=CdT: e.matmul(ps[bank][po:po + 64, 0:128], lhsT=STb[:, h, :],
                                                                                                    rhs=CdT[:, j * 128:(j + 1) * 128], start=False, stop=True),
                                     reads=["STb", n_("CdT")], acc=[f"ps{bank}"], sig=(j % 2 == 1))
                            for yc in range(2):
                                cch = 2 * g + yc
                                P.op("dve", lambda e, tsl=tsl, yc=yc, cch=cch: e.scalar_tensor_tensor(out=yT[:, yc, tsl], in0=xsT[:, yc, tsl],
                                                                                                      scalar=pv[:, PC["sd"] + cch:PC["sd"] + cch + 1],
                                                                                                      in1=ps[3 + yc][:, 0:128], op0=ALU.mult, op1=ALU.add),
                                     reads=[f"ps{3 + yc}", "xsT", "pv"], acc=["yT"])
                        P.op("pe", lambda e, Btok=Btok, xdtw=xdtw: e.matmul(ps[5][:, 128:384], lhsT=Btok[0:Q, :], rhs=xdtw[0:Q, :], start=True, stop=True),
                             reads=[n_("Btok"), n_("xdtw")], writes=["ps5b"])
                        P.op("dve", lambda e, q=q: e.tensor_tensor(out=ST[:, 4 * g:4 * g + 4, :], in0=ST[:, 4 * g:4 * g + 4, :],
                                                                   in1=eend[:, q, 4 * g:4 * g + 4].unsqueeze(2).to_broadcast([128, 4, 64]), op=ALU.mult),
                             reads=["ST", "eend"], writes=["ST"])
                        P.op("dve", lambda e: e.tensor_tensor(out=ST[:, 4 * g:4 * g + 4, :], in0=ST[:, 4 * g:4 * g + 4, :],
                                                              in1=ps[5][:, 128:384].rearrange("p (h d) -> p h d", h=4), op=ALU.add),
                             reads=["ST", "ps5b"], writes=["ST"])
                        P.op("act", lambda e: e.activation(out=STb[:, 4 * g:4 * g + 4, :], in_=ST[:, 4 * g:4 * g + 4, :], func=AF.Copy), reads=["ST"], writes=["STb"])
                    if is_meta:
                        return
                    for yc in range(2):
                        bank = 1 + yc
                        proj(zview, zres, yc, 128, bank)
                        P.op("act", lambda e, bank=bank: e.activation(out=tl[0][:, 0:tb], in_=ps[bank][:, 0:tb], func=AF.Silu), reads=[f"ps{bank}"], writes=["tl0"])
                        P.op("dve", lambda e, yc=yc: e.tensor_tensor(out=yT[:, yc, 0:tb], in0=yT[:, yc, 0:tb], in1=tl[0][:, 0:tb], op=ALU.mult),
                             reads=["yT", "tl0"], writes=["yT"])
                        P.op("act", lambda e, yc=yc: e.activation(out=tl[1 + yc][:, 0:tb], in_=yT[:, yc, 0:tb], func=AF.Square), reads=["yT"], writes=[f"tl{1 + yc}"])
                    for yc in range(2):
                        P.op("pe", lambda e, yc=yc: e.matmul(ps[5][:, 0:tb], lhsT=onesf[:, :], rhs=tl[1 + yc][:, 0:tb], start=(yc == 0), stop=(yc == 1)),
                             reads=[f"tl{1 + yc}", "onesf"], writes=["ps5"] if yc == 0 else [], acc=["ps5"] if yc else [], sig=(yc == 1))
                    P.op("dve", lambda e: e.tensor_scalar(out=tl[3][:, 0:tb], in0=ps[5][:, 0:tb], scalar1=1.0 / 256.0, scalar2=EPS, op0=ALU.mult, op1=ALU.add),
                         reads=["ps5"], writes=["tl3"])
                    P.op("act", lambda e: e.activation(out=tl[3][:, 0:tb], in_=tl[3][:, 0:tb], func=AF.Sqrt), reads=["tl3"], writes=["tl3"])
                    P.op("dve", lambda e: e.reciprocal(out=tl[3][:, 0:tb], in_=tl[3][:, 0:tb]), reads=["tl3"], writes=["tl3"])
                    for yc in range(2):
                        cch = 2 * g + yc
                        P.op("dve", lambda e, yc=yc, cch=cch: e.scalar_tensor_tensor(out=ssdT[:, cch, 0:tb], in0=yT[:, yc, 0:tb],
                                                                                     scalar=pv[:, PC["snw"] + cch:PC["snw"] + cch + 1],
                                                                                     in1=tl[3][:, 0:tb], op0=ALU.mult, op1=ALU.mult),
                             reads=["yT", "tl3", "pv"], acc=["ssdT"])

                def f_z(g, gl_):
                    return lambda view, wres: ssd_group(g, gl_, view, wres)

                for gp in range(4):
                    jobs.append((win_tile(COL_B + gp * 256), 16, WT, f_bc(0, gp)))
                    jobs.append((win_tile(COL_C + gp * 256), 16, WT, f_bc(1, gp)))
                    for gg in range(2):
                        g = gp * 2 + gg
                        jobs.append((win_tile(COL_XS + g * 256), 16, WT, f_xs(g)))
                        if is_meta:
                            jobs.append((None, 0, 0, f_z(g, gg)))
                        else:
                            jobs.append((win_tile(COL_Z + g * 256), 16, WT, f_z(g, gg)))

                if not is_meta:
                    def f_gate(i, which):
                        def fn(view, wres):
                            for j in range(2):
                                dc = i * 2 + j
                                bank = 1 + j
                                proj(view, wres, j, 128, bank)
                                bcol = PC["gb0" if which == 0 else "gb1"] + dc
                                P.op("act", lambda e, bank=bank, j=j, bcol=bcol: e.activation(out=Gs[:, j, 0:tb], in_=ps[bank][:, 0:tb], func=AF.Sigmoid,
                                                                                              bias=pv[:, bcol:bcol + 1]), reads=[f"ps{bank}", "pv"], acc=["Gs"])
                        return fn

                    def f_yo(i, which):
                        src, sres = (ssdT, "ssdT") if which == 0 else (lruT, "lruT")

                        def fn(view, wres):
                            for j in range(2):
                                dc = i * 2 + j
                                bank = 3 + j
                                for k in range(16):
                                    P.op("pe", lambda e, k=k, bank=bank, j=j: e.matmul(ps[bank][:, 0:tb], lhsT=view[:, k, j * 128:(j + 1) * 128],
                                                                                     rhs=src[:, k, 0:tb], start=(k == 0), stop=(k == 15)),
                                         reads=[wres, sres], writes=[f"ps{bank}"] if k == 0 else [], acc=[f"ps{bank}"] if k else [], sig=(k == 15))
                                if which == 0:
                                    P.op("dve", lambda e, j=j, dc=dc, bank=bank: e.tensor_tensor(out=mixT[:, dc, 0:tb], in0=Gs[:, j, 0:tb], in1=ps[bank][:, 0:tb], op=ALU.mult),
                                         reads=["Gs", f"ps{bank}"], acc=["mixT"])
                                else:
                                    P.op("dve", lambda e, j=j, bank=bank: e.tensor_tensor(out=tl[j][:, 0:tb], in0=Gs[:, j, 0:tb], in1=ps[bank][:, 0:tb], op=ALU.mult),
                                         reads=["Gs", f"ps{bank}"], writes=[f"tl{j}"])
                                    P.op("dve", lambda e, j=j, dc=dc: e.tensor_tensor(out=mixT[:, dc, 0:tb], in0=mixT[:, dc, 0:tb], in1=tl[j][:, 0:tb], op=ALU.add),
                                         reads=[f"tl{j}", "mixT"], acc=["mixT"])
                        return fn
                    for i in range(8):
                        jobs.append((win_tile(COL_G0 + i * 256), 16, WT, f_gate(i, 0)))
                        jobs.append((sq_tile(wso_d, i * 256), 16, WT, f_yo(i, 0)))
                        jobs.append((win_tile(COL_G1 + i * 256), 16, WT, f_gate(i, 1)))
                        jobs.append((sq_tile(wlo_d, i * 256), 16, WT, f_yo(i, 1)))

                    def f_o(db):
                        def fn(view, wres):
                            for tt in range(4):
                                tg = blk * 4 + tt
                                xs_ = xsub[tt % 2]
                                xres = f"xsub{tt % 2}"
                                bank = 1 + (tt % 2)
                                P.dma("sp", xres, lambda e, xs_=xs_, tg=tg: e.dma_start(out=xs_[:, :], in_=x_d[tg * 128:(tg + 1) * 128, db * WT:(db + 1) * WT]),
                                      writes=[xres])
                                for k in range(16):
                                    P.op("pe", lambda e, k=k, bank=bank, tt=tt: e.matmul(ps[bank][:, 0:WT], lhsT=mixT[:, k, tt * 128:(tt + 1) * 128], rhs=view[:, k, :],
                                                                                       start=(k == 0), stop=(k == 15)),
                                         reads=[wres, "mixT"], writes=[f"ps{bank}"] if k == 0 else [], acc=[f"ps{bank}"] if k else [], sig=(k == 15))
                                P.op("dve", lambda e, xs_=xs_, bank=bank: e.tensor_tensor(out=xs_[:, :], in0=xs_[:, :], in1=ps[bank][:, 0:WT], op=ALU.add),
                                     reads=[xres, f"ps{bank}"], writes=[xres])
                                P.dma("sp", "h1st" + xres, lambda e, xs_=xs_, tg=tg: e.dma_start(out=h1_d[tg * 128:(tg + 1) * 128, db * WT:(db + 1) * WT], in_=xs_[:, :]),
                                      reads=[xres], acc=["h1d"])
                        return fn
                    for db in range(8):
                        jobs.append((sq_tile(wo_d, db * WT), 16, WT, f_o(db)))

                    def f_route(_v, _r):
                        for tt in range(4):
                            tg = blk * 4 + tt
                            P.dma("sp", "xt", lambda e, tg=tg: e.dma_start(out=xt[:, :], in_=h1_d[tg * 128:(tg + 1) * 128, :]), reads=["h1d"], writes=["xt"])
                            if debug:
                                P.dma("sp", "dbgh1", lambda e, tg=tg: e.dma_start(out=dbg["h1"][tg * 128:(tg + 1) * 128, :], in_=xt[:, :]), reads=["xt"], acc=["dbgh1"])
                            if do_moe:
                                route_tile(tg)
                    jobs.append((None, 0, 0, f_route))

                _km = int(os.environ.get("KMAXJOBS", "100000"))
                run_jobs(ring, jobs[:_km])

            def route_tile(tg):
                rms_rstd(xt[:, :], 128, xn[:, :], "xn", 1, "xt")
                P.op("dve", lambda e: e.scalar_tensor_tensor(out=xt[:, :], in0=xt[:, :], scalar=sm[:, 1:2], in1=gffn[:, :], op0=ALU.mult, op1=ALU.mult),
                     reads=["xt", "sm", "gffn"], writes=["xt"])
                P.op("act", lambda e: e.activation(out=xn[:, :], in_=xt[:, :], func=AF.Copy), reads=["xt"], writes=["xn"])
                for hf in range(2):
                    for qd in range(2):
                        for j in range(4):
                            k = hf * 8 + qd * 4 + j
                            P.op("pe", lambda e, k=k, j=j: e.transpose(out=ps[5][:, j * 128:(j + 1) * 128], in_=xt[:, k * 128:(k + 1) * 128], identity=identf[:, :]),
                                 reads=["xt", "identf"], writes=["ps5"] if j == 0 else [], acc=["ps5"] if j else [], sig=(j == 3))
                        P.op("act", lambda e, qd=qd: e.activation(out=(u2T, u2Tb)[qd][:, :, :], in_=ps[5][:, :].rearrange("p (j t) -> p j t", j=4), func=AF.Copy),
                             reads=["ps5"], writes=[f"tl{4 + qd}"])
                    for kk in range(8):
                        k = hf * 8 + kk
                        P.op("pe", lambda e, k=k, kk=kk: e.matmul(ps[6][:, 0:72], lhsT=(u2T, u2Tb)[kk // 4][:, kk % 4, :], rhs=wr[:, k, :], start=(k == 0), stop=(k == 15)),
                             reads=["tl4", "tl5", "wr"], writes=["ps6"] if k == 0 else [], acc=["ps6"] if k else [], sig=(kk == 7))
                lg = rt[:, 0, :]
                tmp = rt[:, 1, 0:64]
                esel, oh1, es2, oh2, goh = rt[:, 2, 0:8], rt[:, 2, 8:16], rt[:, 2, 16:24], rt[:, 2, 24:32], rt[:, 2, 32:40]
                A1, A2, Asum, pos = rt[:, 3, 0:64], rt[:, 4, 0:64], rt[:, 5, 0:64], rt[:, 6, 0:64]
                s = lambda c: sm[:, c:c + 1]
                RT = ["rt"]

                def dv(fn, extra=()):
                    wx = [x for x in extra if x in ("IDX", "GATE")]
                    P.op("dve", fn, reads=RT + list(extra), writes=RT, acc=wx)

                P.op("act", lambda e: e.activation(out=lg, in_=ps[6][:, 0:72], func=AF.Copy), reads=["ps6"], writes=RT)
                dv(lambda e: e.reduce_max(out=s(8), in_=lg[:, 0:8], axis=AX.X))
                dv(lambda e: e.tensor_scalar(out=goh, in0=lg[:, 0:8], scalar1=s(8), scalar2=None, op0=ALU.is_equal))
                dv(lambda e: e.tensor_scalar(out=s(9), in0=s(8), scalar1=-1.0, scalar2=None, op0=ALU.mult))
                P.op("act", lambda e: e.activation(out=tmp[:, 0:8], in_=lg[:, 0:8], func=AF.Exp, bias=s(9), accum_out=s(10)), reads=RT, writes=RT)
                dv(lambda e: e.reciprocal(out=s(10), in_=s(10)))
                dv(lambda e: e.tensor_tensor(out=tmp.rearrange("p (g x) -> p g x", g=8), in0=lg[:, 8:72].rearrange("p (g x) -> p g x", g=8),
                                             in1=goh.unsqueeze(2).to_broadcast([128, 8, 8]), op=ALU.mult))
                dv(lambda e: e.reduce_sum(out=esel, in_=tmp.rearrange("p (g x) -> p x g", g=8), axis=AX.X))
                dv(lambda e: e.reduce_max(out=s(11), in_=esel, axis=AX.X))
                dv(lambda e: e.tensor_scalar(out=oh1, in0=esel, scalar1=s(11), scalar2=None, op0=ALU.is_equal))
                dv(lambda e: e.scalar_tensor_tensor(out=es2, in0=oh1, scalar=-1e30, in1=esel, op0=ALU.mult, op1=ALU.add))
                dv(lambda e: e.reduce_max(out=s(12), in_=es2, axis=AX.X))
                dv(lambda e: e.tensor_scalar(out=oh2, in0=es2, scalar1=s(12), scalar2=None, op0=ALU.is_equal))
                dv(lambda e: e.tensor_tensor(out=s(13), in0=s(12), in1=s(11), op=ALU.subtract))
                P.op("act", lambda e: e.activation(out=s(13), in_=s(13), func=AF.Exp), reads=RT, writes=RT)
                dv(lambda e: e.tensor_scalar(out=s(14), in0=s(13), scalar1=1.0, scalar2=None, op0=ALU.add))
                dv(lambda e: e.reciprocal(out=s(14), in_=s(14)))
                dv(lambda e: e.tensor_tensor(out=GATE[:, tg, 0:1], in0=s(10), in1=s(14), op=ALU.mult), extra=["GATE"])
                dv(lambda e: e.tensor_tensor(out=GATE[:, tg, 1:2], in0=GATE[:, tg, 0:1], in1=s(13), op=ALU.mult), extra=["GATE"])
                dv(lambda e: e.tensor_tensor(out=A1.rearrange("p (g x) -> p g x", g=8), in0=goh.unsqueeze(2).to_broadcast([128, 8, 8]),
                                             in1=oh1.unsqueeze(1).to_broadcast([128, 8, 8]), op=ALU.mult))
                dv(lambda e: e.tensor_tensor(out=A2.rearrange("p (g x) -> p g x", g=8), in0=goh.unsqueeze(2).to_broadcast([128, 8, 8]),
                                             in1=oh2.unsqueeze(1).to_broadcast([128, 8, 8]), op=ALU.mult))
                dv(lambda e: e.tensor_tensor(out=Asum, in0=A1, in1=A2, op=ALU.add))
                P.op("pe", lambda e: e.matmul(ps[7][:, 0:64], lhsT=triS[:, :], rhs=Asum, start=True, stop=True), reads=RT + ["triS"], writes=["ps7"])
                P.op("pe", lambda e: e.matmul(ps[7][:, 64:128], lhsT=onesf[:, :], rhs=Asum, start=True, stop=True), reads=RT + ["onesf"], acc=["ps7"])
                dv(lambda e: e.tensor_tensor(out=pos, in0=ps[7][:, 0:64], in1=basebc[:, :], op=ALU.add), extra=["ps7", "basebc"])
                P.op("dve", lambda e: e.tensor_tensor(out=basebc[:, :], in0=basebc[:, :], in1=ps[7][:, 64:128], op=ALU.add), reads=["ps7", "basebc"] + RT, writes=["basebc"])
                dv(lambda e: e.tensor_tensor(out=pos, in0=pos, in1=eoff[:, :], op=ALU.add), extra=["eoff"])
                for kk, Ak in ((0, A1), (1, A2)):
                    dv(lambda e, Ak=Ak: e.tensor_tensor(out=tmp, in0=Ak, in1=pos, op=ALU.mult))
                    dv(lambda e, kk=kk: e.reduce_sum(out=s(16 + kk), in_=tmp, axis=AX.X))
                    dv(lambda e, kk=kk: e.tensor_copy(out=IDX[:, tg, kk:kk + 1], in_=s(16 + kk)), extra=["IDX"])
                    P.dma("pool", f"scat{kk}", lambda e, kk=kk: e.indirect_dma_start(
                        out=xb_d[:, :], out_offset=bass.IndirectOffsetOnAxis(ap=IDX[:, tg, kk:kk + 1], axis=0), in_=xn[:, :], in_offset=None,
                        bounds_check=NE * CAP - 1, oob_is_err=False), reads=["xn", "IDX", "xbz"], acc=["xbuf"])
                if debug:
                    dv(lambda e: e.tensor_copy(out=rt[:, 8, 0:1], in_=s(16)))
                    dv(lambda e: e.tensor_copy(out=rt[:, 8, 1:2], in_=s(17)))
                    dv(lambda e: e.tensor_copy(out=rt[:, 8, 2:4], in_=GATE[:, tg, :]))
                    P.dma("sp", "dbgrt", lambda e: e.dma_start(out=dbg["route"][:, tg, :], in_=rt[:, 8, 0:4]), reads=RT, acc=["dbgrt"])

            if do_moe:
                P.op("dve", lambda e: e.memset(mixT[:, 0:4, :], 0.0), writes=["mixT"])
                for e_ in range(NE):
                    P.dma("sp", "xbz", lambda e, e_=e_: e.dma_start(out=xb_d[e_ * CAP:(e_ + 1) * CAP, :], in_=mixT[:, 0:4, :].rearrange("p a b -> p (a b)")),
                          reads=["mixT"], acc=["xbz"])

            mixer_block(-1)
            for blk in range(nblk):
                mixer_block(blk)
                if debug and blk == 0:
                    for nm, src in (("ssdT", ssdT), ("lruT", lruT), ("mixT", mixT)):
                        for k in range(16):
                            P.op("act", lambda e, k=k, src=src: e.activation(out=tl[0][:, :], in_=src[:, k, :], func=AF.Copy), reads=[nm], writes=["tl0"])
                            P.dma("sp", "dbg" + nm, lambda e, k=k, nm=nm: e.dma_start(out=dbg[nm][:, k, :], in_=tl[0][:, :]), reads=["tl0"], acc=["dbg" + nm])
            P.barrier()

        if do_moe:
            with ExitStack() as st3:
                sb3 = lambda name, shape, dt: st3.enter_context(nc.sbuf_tensor("s_" + name, shape, dt))
                ring3 = Ring(P, [sb3(f"esl{i}", [128, 8192], BF16) for i in range(4)], "ew")
                Xe = [sb3(f"Xe{i}", [128, D], BF16) for i in range(2)]
                XeT = sb3("XeT", [128, 16, 128], BF16)
                sa = sb3("sa", [128, 512], F32)
                hb = sb3("hb", [128, 512], BF16)
                hT = sb3("hT", [128, 4, 128], BF16)
                ysb = [sb3(f"ysb{i}", [128, D], F32) for i in range(2)]
                gfin = sb3("gfin", [128, D], F32)
                hh = [sb3(f"hh{i}", [128, D], F32) for i in range(2)]
                y1 = [sb3(f"y1_{i}", [128, D], F32) for i in range(2)]
                y2 = [sb3(f"y2_{i}", [128, D], F32) for i in range(2)]
                sm3 = sb3("sm3", [128, 8], F32)
                P.dma("sp", "c_gfin", lambda e: e.dma_start(out=gfin[:], in_=gfin_d), writes=["gfin"])
                jobs = []

                def f_e(e_, fb, kind):
                    xi = e_ % 2

                    def fn(view, wres):
                        if kind == "w1":
                            if fb == 0:
                                P.dma("sp", f"Xe{xi}", lambda e: e.dma_start(out=Xe[xi][:, :], in_=xb_d[e_ * CAP:(e_ + 1) * CAP, :]), reads=["xbuf", "xbz"], writes=[f"Xe{xi}"])
                                for half in range(2):
                                    for j in range(8):
                                        k = half * 8 + j
                                        P.op("pe", lambda e, k=k, j=j: e.transpose(out=ps0[:, j * 128:(j + 1) * 128], in_=Xe[xi][:, k * 128:(k + 1) * 128], identity=identb[:, :]),
                                             reads=[f"Xe{xi}", "identb"], writes=["ps0"] if j == 0 else [], acc=["ps0"] if j else [], sig=(j == 7))
                                    P.op("dve", lambda e, half=half: e.tensor_copy(out=XeT[:, half * 8:half * 8 + 8, :], in_=ps0[:, :].rearrange("p (j t) -> p j t", j=8)),
                                         reads=["ps0"], writes=["XeT"] if half == 0 else [], acc=["XeT"] if half else [])
                        if kind in ("w1", "w3"):
                            bank = 5 if kind == "w1" else 6
                            for k in range(16):
                                P.op("pe", lambda e, k=k: e.matmul(ps[bank][:, :], lhsT=XeT[:, k, :], rhs=view[:, k, :], start=(k == 0), stop=(k == 15)),
                                     reads=[wres, "XeT"], writes=[f"ps{bank}"] if k == 0 else [], acc=[f"ps{bank}"] if k else [], sig=(k == 15))
                            if kind == "w1":
                                P.op("act", lambda e: e.activation(out=sa[:, :], in_=ps[5][:, :], func=AF.Silu), reads=["ps5"], writes=["sa"])
                            else:
                                P.op("dve", lambda e: e.tensor_tensor(out=hb[:, :], in0=sa[:, :], in1=ps[6][:, :], op=ALU.mult), reads=["sa", "ps6"], writes=["hb"])
                                for j in range(4):
                                    P.op("pe", lambda e, j=j: e.transpose(out=ps0[:, j * 128:(j + 1) * 128], in_=hb[:, j * 128:(j + 1) * 128], identity=identb[:, :]),
                                         reads=["hb", "identb"], writes=["ps0"] if j == 0 else [], acc=["ps0"] if j else [], sig=(j == 3))
                                P.op("dve", lambda e: e.tensor_copy(out=hT[:, :, :], in_=ps0[:, 0:512].rearrange("p (j t) -> p j t", j=4)), reads=["ps0"], writes=["hT"])
                            return
                        for d4 in range(4):
                            bank = 1 + d4
                            for ffc in range(4):
                                first = (fb == 0 and ffc == 0)
                                last = (fb == 1 and ffc == 3)
                                P.op("pe", lambda e, ffc=ffc, d4=d4, bank=bank, first=first, last=last: e.matmul(
                                    ps[bank][:, :], lhsT=hT[:, ffc, :], rhs=view[:, ffc, d4 * 512:(d4 + 1) * 512], start=first, stop=last),
                                    reads=[wres, "hT"], writes=[f"ps{bank}"] if first else [], acc=[] if first else [f"ps{bank}"], sig=(ffc == 3))
                        if fb == 1:
                            yi = e_ % 2
                            for d4 in range(4):
                                if d4 % 2 == 0:
                                    P.op("act", lambda e, d4=d4: e.activation(out=ysb[yi][:, d4 * 512:(d4 + 1) * 512], in_=ps[1 + d4][:, :], func=AF.Copy),
                                         reads=[f"ps{1 + d4}"], acc=[f"ysb{yi}"])
                                else:
                                    P.op("dve", lambda e, d4=d4: e.tensor_copy(out=ysb[yi][:, d4 * 512:(d4 + 1) * 512], in_=ps[1 + d4][:, :]),
                                         reads=[f"ps{1 + d4}"], acc=[f"ysb{yi}"])
                            P.dma("sp", f"yst{yi}", lambda e: e.dma_start(out=yb_d[e_ * CAP:(e_ + 1) * CAP, :], in_=ysb[yi][:, :]), reads=[f"ysb{yi}"], acc=["ybuf"])
                    return fn

                for e_ in range(NE):
                    for fb in range(2):
                        jobs.append(([(0, 16, w1_d[e_].rearrange("(k p) f -> p k f", p=128)[:, :, fb * 512:(fb + 1) * 512])], 16, 512, f_e(e_, fb, "w1")))
                        jobs.append(([(0, 16, w3_d[e_].rearrange("(k p) f -> p k f", p=128)[:, :, fb * 512:(fb + 1) * 512])], 16, 512, f_e(e_, fb, "w3")))
                        jobs.append(([(0, 4, w2_d[e_][fb * 512:(fb + 1) * 512, :].rearrange("(k p) n -> p k n", p=128))], 4, 2048, f_e(e_, fb, "w2")))
                run_jobs(ring3, jobs)

                for tg in range(16):
                    b = tg % 2
                    P.dma("sp", f"hh{b}", lambda e, b=b, tg=tg: e.dma_start(out=hh[b][:, :], in_=h1_d[tg * 128:(tg + 1) * 128, :]), reads=["h1d"], writes=[f"hh{b}"])
                    for kk, yy in ((0, y1), (1, y2)):
                        P.dma("pool", f"g{kk}_{b}", lambda e, kk=kk, yy=yy, b=b, tg=tg: e.indirect_dma_start(
                            out=yy[b][:, :], out_offset=None, in_=yb_d[:, :], in_offset=bass.IndirectOffsetOnAxis(ap=IDX[:, tg, kk:kk + 1], axis=0)),
                            reads=["ybuf", "IDX"], writes=[f"y{kk}_{b}"])
                        P.op("dve", lambda e, kk=kk, yy=yy, b=b, tg=tg: e.scalar_tensor_tensor(out=hh[b][:, :], in0=yy[b][:, :], scalar=GATE[:, tg, kk:kk + 1],
                                                                                               in1=hh[b][:, :], op0=ALU.mult, op1=ALU.add),
                             reads=[f"y{kk}_{b}", "GATE", f"hh{b}"], writes=[f"hh{b}"])
                    P.op("act", lambda e, b=b: e.activation(out=y1[b][:, :], in_=hh[b][:, :], func=AF.Square, accum_out=sm3[:, 0:1]), reads=[f"hh{b}"], writes=[f"y0_{b}", "sm3"])
                    P.op("dve", lambda e: e.tensor_scalar(out=sm3[:, 0:1], in0=sm3[:, 0:1], scalar1=1.0 / D, scalar2=EPS, op0=ALU.mult, op1=ALU.add), reads=["sm3"], writes=["sm3"])
                    P.op("act", lambda e: e.activation(out=sm3[:, 0:1], in_=sm3[:, 0:1], func=AF.Sqrt), reads=["sm3"], writes=["sm3"])
                    P.op("dve", lambda e: e.reciprocal(out=sm3[:, 0:1], in_=sm3[:, 0:1]), reads=["sm3"], writes=["sm3"])
                    P.op("dve", lambda e, b=b: e.scalar_tensor_tensor(out=y2[b][:, :], in0=hh[b][:, :], scalar=sm3[:, 0:1], in1=gfin[:, :], op0=ALU.mult, op1=ALU.mult),
                         reads=[f"hh{b}", "sm3", "gfin"], writes=[f"y1_{b}"])
                    P.dma("sp", f"ost{b}", lambda e, b=b, tg=tg: e.dma_start(out=out_d[tg * 128:(tg + 1) * 128, :], in_=y2[b][:, :]), reads=[f"y1_{b}"], acc=["outd"])
                P.wait_all("sp", ["outd"])
        else:
            P.wait_all("sp", ["h1d"])
        if debug:
            P.wait_all("sp", ["dbgh1", "dbgssdT", "dbglruT", "dbgmixT"] + (["dbgrt"] if do_moe else []))
        P.emit()
    return nc


def _pack_params(inp):
    pv = np.zeros((128, NPC), np.float32)

    def put(name, vec, col=0):
        v = np.asarray(vec, np.float32).reshape(-1, 128).T
        pv[:, PC[name] + col:PC[name] + col + v.shape[1]] = v
    put("gmix", inp["norm_mix"][0])
    for k in range(4):
        put("scw", inp["ssd_conv_w"][0, k], k * 32)
        put("lcw", inp["lru_conv_w"][0, k], k * 16)
    put("scb", inp["ssd_conv_b"][0])
    put("snw", inp["ssd_norm"][0])
    put("sd", np.repeat(np.asarray(inp["ssd_d"][0], np.float32), 64))
    put("lcb", inp["lru_conv_b"][0])
    put("lba", inp["lru_ba"][0])
    put("lbx", inp["lru_bx"][0])
    put("llam", inp["lru_lambda"][0])
    put("gb0", inp["gate_bias"][0, 0])
    put("gb1", inp["gate_bias"][0, 1])
    pv[0:32, PC["dtb"]] = np.asarray(inp["ssd_dt_bias"][0], np.float32)
    return pv


def make_in_maps(inp, cores):
    f = lambda a: np.ascontiguousarray(np.asarray(a, np.float32))
    shared = {
        "meta": np.ascontiguousarray(np.concatenate([np.zeros((128 - NMETA, D), np.float32), f(inp["meta_tokens"])], axis=0)),
        "pvec": _pack_params(inp),
        "alog_bc": np.ascontiguousarray(np.broadcast_to(np.asarray(inp["ssd_a_log"][0], np.float32)[None, :], (128, 32))),
        "gffn_bc": np.ascontiguousarray(np.broadcast_to(np.asarray(inp["norm_ffn"][0], np.float32)[None, :], (128, D))),
        "gfin_bc": np.ascontiguousarray(np.broadcast_to(np.asarray(inp["norm_final"], np.float32)[None, :], (128, D))),
        "wr": np.ascontiguousarray(np.concatenate([np.asarray(inp["w_router_group"][0], np.float32), np.asarray(inp["w_router_expert"][0], np.float32)], axis=1)),
        "w_in": f(inp["w_in"][0]),
        "w_ssd_out": f(inp["w_ssd_out"][0]),
        "w_lru_out": f(inp["w_lru_out"][0]),
        "w_out": f(inp["w_out"][0]),
        "lru_wa": f(inp["lru_wa"][0]),
        "lru_wx": f(inp["lru_wx"][0]),
        "w1": f(inp["w_exp_gate"][0]),
        "w3": f(inp["w_exp_up"][0]),
        "w2": f(inp["w_exp_down"][0]),
    }
    maps = []
    for c in cores:
        m = dict(shared)
        m["x"] = f(inp["x"][c])
        maps.append(m)
    return maps


def kernel(**inputs):
    nc = build()
    cores = list(range(8))
    in_maps = make_in_maps(inputs, cores)
    res = run_bass_kernel_spmd(nc, in_maps, core_ids=cores)
    return np.stack([np.asarray(r["out"], np.float32) for r in res.results], axis=0)
```
